# Optimizing a Trainium2 kernel written in Bass

```python
import jax, jax.numpy as jnp
from jax import lax
import numpy as np

D_MODEL = 4096
BATCH = 4
SEQ = 2048
DEPTH = 1
DEC_BATCH = 128
DEC_SEQ = 8
PAST_LEN = 16384
PAGE_SIZE = 128

N_META = 16
W_A = D_MODEL // 2
K_A = 3
W_R = D_MODEL // 2
K_R = 4
LRU_HEADS = 16
LRU_BLOCK = W_R // LRU_HEADS
C_RG = 8.0
N_GROUPS = 4
EXPERTS_PER_GROUP = 8
N_EXPERTS = N_GROUPS * EXPERTS_PER_GROUP
TOP_K = 2
D_EXPERT = D_MODEL // 8
EPS = 1e-6
SPLIT_SIZES = (W_A, W_A, W_A, W_R, W_R, D_MODEL, D_MODEL)
N_IN = sum(SPLIT_SIZES)

kernel_name = "hybrid_shortconv_rglru_hmoe_step"


def rmsnorm(x, g):
    xf = x.astype(jnp.float32)
    y = xf * lax.rsqrt(jnp.mean(xf * xf, axis=-1, keepdims=True) + EPS)
    return (y * g.astype(jnp.float32)).astype(x.dtype)


def causal_dwconv(u, buf, w):
    k = w.shape[0]
    t = u.shape[1]
    full = jnp.concatenate([buf.astype(u.dtype), u], axis=1)
    out = full[:, 0:t] * w[0]
    for j in range(1, k):
        out = out + full[:, j:j + t] * w[j]
    return out, full[:, full.shape[1] - (k - 1):]


def block_diag(x, w, b):
    n, t, _ = x.shape
    xb = x.reshape(n, t, LRU_HEADS, LRU_BLOCK)
    return jnp.einsum('nthi,hij->nthj', xb, w).reshape(n, t, W_R) + b


def rg_lru(x, h0, w_a, b_a, w_x, b_x, lam):
    r = jax.nn.sigmoid(block_diag(x, w_a, b_a).astype(jnp.float32))
    i = jax.nn.sigmoid(block_diag(x, w_x, b_x).astype(jnp.float32))
    log_a = -C_RG * r * jax.nn.softplus(-lam.astype(jnp.float32))
    a = jnp.exp(log_a)
    bterm = jnp.sqrt(-jnp.expm1(2.0 * log_a)) * i * x.astype(jnp.float32)

    def step(h, ab):
        a_t, b_t = ab
        h = a_t * h + b_t
        return h, h

    h_last, hs = lax.scan(step, h0.astype(jnp.float32), (jnp.swapaxes(a, 0, 1), jnp.swapaxes(bterm, 0, 1)))
    return jnp.swapaxes(hs, 0, 1).astype(x.dtype), h_last.astype(h0.dtype)


def hier_moe(x, w_group, b_group, w_router, b_router, w_gate, w_up, w_down):
    n, t, d = x.shape
    xt = x.reshape(n * t, d)
    m = xt.shape[0]
    g_prob = jax.nn.softmax((xt @ w_group).astype(jnp.float32) + b_group.astype(jnp.float32), axis=-1)
    g_p, g_idx = lax.top_k(g_prob, 1)
    e_logits = ((xt @ w_router).astype(jnp.float32) + b_router.astype(jnp.float32)).reshape(m, N_GROUPS, EXPERTS_PER_GROUP)
    e_sel = jnp.take_along_axis(e_logits, g_idx[:, :, None], axis=1)[:, 0]
    e_p, e_idx = lax.top_k(jax.nn.softmax(e_sel, axis=-1), TOP_K)
    weights = e_p / jnp.sum(e_p, axis=-1, keepdims=True) * g_p
    expert_id = g_idx * EXPERTS_PER_GROUP + e_idx
    gates = jnp.sum(jax.nn.one_hot(expert_id, N_EXPERTS, dtype=jnp.float32) * weights[..., None], axis=1)
    gates = gates.astype(x.dtype)
    out = jnp.zeros_like(xt)
    for e in range(N_EXPERTS):
        h = jax.nn.silu(xt @ w_gate[e]) * (xt @ w_up[e])
        out = out + gates[:, e:e + 1] * (h @ w_down[e])
    return out.reshape(n, t, d)


def trunk_layer(x, buf_a, buf_r, h0, norm1, w_in, conv_a_w, conv_r_w, conv_r_b, lru_wa, lru_ba, lru_wx, lru_bx,
                lru_lam, w_br_a, w_br_r, w_o, norm2, w_group, b_group, w_router, b_router, w_gate, w_up, w_down):
    u = rmsnorm(x, norm1)
    z = u @ w_in
    idx = list(np.cumsum(SPLIT_SIZES)[:-1])
    b_a, c_a, v_a, x_r, y_r, g_a, g_r = jnp.split(z, idx, axis=-1)
    conv_a_out, new_buf_a = causal_dwconv(c_a * v_a, buf_a, conv_a_w)
    out_a = b_a * conv_a_out
    xr_c, new_buf_r = causal_dwconv(x_r, buf_r, conv_r_w)
    xr_c = xr_c + conv_r_b
    h_seq, h_last = rg_lru(xr_c, h0, lru_wa, lru_ba, lru_wx, lru_bx, lru_lam)
    out_r = h_seq * jax.nn.gelu(y_r)
    merged = jax.nn.sigmoid(g_a) * (out_a @ w_br_a) + jax.nn.sigmoid(g_r) * (out_r @ w_br_r)
    x = x + merged @ w_o
    x = x + hier_moe(rmsnorm(x, norm2), w_group, b_group, w_router, b_router, w_gate, w_up, w_down)
    return x, new_buf_a, new_buf_r, h_last


def setup_inputs(seed: int = 0) -> dict:
    key = jax.random.key(seed)
    ks = jax.random.split(key, 32)
    f32 = jnp.float32
    nrm = lambda k, shape, s: jax.random.normal(k, shape, f32) * s
    u = jax.random.uniform(ks[13], (DEPTH, W_R), f32, 0.9, 0.999)
    s = u ** (1.0 / C_RG)
    lam = jnp.log(s / (1.0 - s))
    return {
        "x_prompt": nrm(ks[0], (BATCH, SEQ, D_MODEL), 1.0),
        "x_sample": nrm(ks[1], (DEC_BATCH, DEC_SEQ, D_MODEL), 1.0),
        "state_conv_a": nrm(ks[2], (DEPTH, DEC_BATCH, K_A - 1, W_A), 1.0),
        "state_conv_r": nrm(ks[3], (DEPTH, DEC_BATCH, K_R - 1, W_R), 1.0),
        "state_h": nrm(ks[4], (DEPTH, DEC_BATCH, W_R), 0.5),
        "meta_tokens": nrm(ks[5], (N_META, D_MODEL), 1.0),
        "norm1": 1.0 + nrm(ks[6], (DEPTH, D_MODEL), 0.02),
        "w_in": nrm(ks[7], (DEPTH, D_MODEL, N_IN), D_MODEL ** -0.5),
        "conv_a_w": nrm(ks[8], (DEPTH, K_A, W_A), K_A ** -0.5),
        "conv_r_w": nrm(ks[9], (DEPTH, K_R, W_R), K_R ** -0.5),
        "conv_r_b": nrm(ks[10], (DEPTH, W_R), 0.02),
        "lru_wa": nrm(ks[11], (DEPTH, LRU_HEADS, LRU_BLOCK, LRU_BLOCK), LRU_BLOCK ** -0.5),
        "lru_ba": nrm(ks[12], (DEPTH, W_R), 0.02),
        "lru_wx": nrm(ks[14], (DEPTH, LRU_HEADS, LRU_BLOCK, LRU_BLOCK), LRU_BLOCK ** -0.5),
        "lru_bx": nrm(ks[15], (DEPTH, W_R), 0.02),
        "lru_lam": lam,
        "w_br_a": nrm(ks[16], (DEPTH, W_A, D_MODEL), W_A ** -0.5),
        "w_br_r": nrm(ks[17], (DEPTH, W_R, D_MODEL), W_R ** -0.5),
        "w_o": nrm(ks[18], (DEPTH, D_MODEL, D_MODEL), D_MODEL ** -0.5),
        "norm2": 1.0 + nrm(ks[19], (DEPTH, D_MODEL), 0.02),
        "w_group": nrm(ks[20], (DEPTH, D_MODEL, N_GROUPS), D_MODEL ** -0.5),
        "b_group": nrm(ks[21], (DEPTH, N_GROUPS), 0.01),
        "w_router": nrm(ks[22], (DEPTH, D_MODEL, N_EXPERTS), D_MODEL ** -0.5),
        "b_router": nrm(ks[23], (DEPTH, N_EXPERTS), 0.01),
        "w_gate": nrm(ks[24], (DEPTH, N_EXPERTS, D_MODEL, D_EXPERT), D_MODEL ** -0.5),
        "w_up": nrm(ks[25], (DEPTH, N_EXPERTS, D_MODEL, D_EXPERT), D_MODEL ** -0.5),
        "w_down": nrm(ks[26], (DEPTH, N_EXPERTS, D_EXPERT, D_MODEL), D_EXPERT ** -0.5),
        "norm_f": 1.0 + nrm(ks[27], (D_MODEL,), 0.02),
    }


def reference(x_prompt, x_sample, state_conv_a, state_conv_r, state_h, meta_tokens, norm1, w_in, conv_a_w, conv_r_w,
              conv_r_b, lru_wa, lru_ba, lru_wx, lru_bx, lru_lam, w_br_a, w_br_r, w_o, norm2, w_group, b_group,
              w_router, b_router, w_gate, w_up, w_down, norm_f):
    bp = x_prompt.shape[0]
    meta = jnp.broadcast_to(meta_tokens[None].astype(x_prompt.dtype), (bp, N_META, D_MODEL))
    xp = jnp.concatenate([meta, x_prompt], axis=1)
    xs = x_sample
    zero_a = jnp.zeros((bp, K_A - 1, W_A), x_prompt.dtype)
    zero_r = jnp.zeros((bp, K_R - 1, W_R), x_prompt.dtype)
    zero_h = jnp.zeros((bp, W_R), state_h.dtype)
    pa, pr, ph, sa, sr, sh = [], [], [], [], [], []
    for l in range(DEPTH):
        lw = (norm1[l], w_in[l], conv_a_w[l], conv_r_w[l], conv_r_b[l], lru_wa[l], lru_ba[l], lru_wx[l], lru_bx[l],
              lru_lam[l], w_br_a[l], w_br_r[l], w_o[l], norm2[l], w_group[l], b_group[l], w_router[l], b_router[l],
              w_gate[l], w_up[l], w_down[l])
        xp, ba, br, hl = trunk_layer(xp, zero_a, zero_r, zero_h, *lw)
        pa.append(ba); pr.append(br); ph.append(hl)
        xs, ba, br, hl = trunk_layer(xs, state_conv_a[l], state_conv_r[l], state_h[l], *lw)
        sa.append(ba); sr.append(br); sh.append(hl)
    y_prompt = rmsnorm(xp, norm_f)[:, N_META:]
    y_sample = rmsnorm(xs, norm_f)
    return (y_prompt, y_sample, jnp.stack(pa), jnp.stack(pr), jnp.stack(ph), jnp.stack(sa), jnp.stack(sr), jnp.stack(sh))
```

```python
import contextlib
import numpy as np
import concourse.bass as bass
import concourse.mybir as mybir
from concourse.bass_utils import run_bass_kernel_spmd

F32 = mybir.dt.float32
BF16 = mybir.dt.bfloat16
AF = mybir.ActivationFunctionType
ALU = mybir.AluOpType
AX = mybir.AxisListType

D = 4096
W = 2048
NCH = 16
KD = 32
N = 608
NT = 304
GS = [128, 128, 128, 128, 96]
NG = 5
NE = 32
DE = 512
EPS = 1e-6
SB0 = 430
NCORES = 8
RING = 4
NPS = 6
GRAN = 4096


class Buf:
    __slots__ = ("lw", "rd", "dsem", "dcnt", "name")

    def __init__(self, name=""):
        self.lw = None
        self.rd = []
        self.dsem = None
        self.dcnt = 0
        self.name = name


class Eng:
    def __init__(self, name, sem, is_pe=False, is_dma=False):
        self.name = name
        self.sem = sem
        self.seq = 0
        self.items = []
        self.waited = {}
        self.is_pe = is_pe
        self.is_dma = is_dma


class Sched:
    def __init__(self, nc, es):
        self.nc = nc
        self.es = es
        self.nsem = 0
        self.engs = {}
        for nm in ("pe", "act", "dve", "sp", "pool"):
            self.engs[nm] = Eng(nm, self.newsem(nm), is_pe=(nm == "pe"), is_dma=nm in ("sp", "pool"))
        self.pending_dma = []

    def newsem(self, name):
        self.nsem += 1
        return self.es.enter_context(self.nc.semaphore("s_%s_%d" % (name, self.nsem)))

    def _wait(self, eng, ev):
        key, sem, val = ev
        if eng.waited.get(key, 0) >= val:
            return
        eng.waited[key] = val
        eng.items.append(("w", sem, val))

    def _deps(self, eng, reads, writes):
        for b in reads:
            if b.lw is not None:
                self._dep(eng, b.lw, raw=True)
        for b in writes:
            if b.lw is not None:
                self._dep(eng, b.lw, raw=True)
            for ev in b.rd:
                self._dep(eng, ev, raw=False)

    def _dep(self, eng, ev, raw):
        key = ev[0]
        if key == eng.name:
            if eng.is_pe:
                return
        self._wait(eng, ev)

    def op(self, engname, fn, reads=(), writes=()):
        eng = self.engs[engname]
        self._deps(eng, reads, writes)
        eng.seq += 1
        ev = (eng.name, eng.sem, eng.seq)
        eng.items.append(("o", fn, eng.sem, 1))
        for b in reads:
            b.rd.append(ev)
        for b in writes:
            b.lw = ev
            b.rd = []
        return ev

    def dma(self, engname, out, in_, owner, reads=(), writes=(), nodeps=False):
        eng = self.engs[engname]
        if owner.dsem is None:
            owner.dsem = self.newsem("d")
        if not nodeps:
            self._deps(eng, reads, writes)
        owner.dcnt += 16
        ev = ("d%d" % id(owner), owner.dsem, owner.dcnt)
        eng.items.append(("o", lambda e, o=out, i=in_: e.dma_start(out=o, in_=i), owner.dsem, 16))
        for b in reads:
            b.rd.append(ev)
        for b in writes:
            b.lw = ev
            b.rd = []
        if engname == "sp":
            self.pending_dma.append(ev)
        return ev

    def barrier(self):
        for nm in ("pe", "act", "dve", "sp"):
            e = self.engs[nm]
            for nm2 in ("pe", "act", "dve"):
                e2 = self.engs[nm2]
                if nm2 != nm and e2.seq > 0:
                    self._wait(e, (e2.name, e2.sem, e2.seq))
            for ev in self.pending_dma:
                self._wait(e, ev)
        self.pending_dma = []

    def replay(self, engname, h):
        for it in self.engs[engname].items:
            if it[0] == "w":
                h.wait_ge(it[1], it[2])
            else:
                ins = it[1](h)
                ins.then_inc(it[2], it[3])


def build_program():
    nc = bass.Bass("TRN2", target_bir_lowering=False)
    es = contextlib.ExitStack()
    with es:
        _build(nc, es)
    return nc


def _build(nc, es):
    def din(name, shape):
        return nc.dram_tensor(name, list(shape), F32, kind="ExternalInput").ap()

    def dout(name, shape):
        return nc.dram_tensor(name, list(shape), F32, kind="ExternalOutput").ap()

    xpre = din("xpre", [2 * N, D])
    xin = din("xin", [2 * N, D])
    import os
    _tiny = bool(os.environ.get("MK_TINY"))
    w_in = din("w_in", [144, 128, GRAN])
    w_bra = din("w_br_a", [128 if _tiny else W, D])
    w_brr = din("w_br_r", [128 if _tiny else W, D])
    w_o = din("w_o", [128 if _tiny else D, D])
    import os
    _ne = 1 if os.environ.get("MK_SMALL") else NE
    w_gate = din("w_gate", [_ne, 4, 128, GRAN])
    w_up = din("w_up", [_ne, 4, 128, GRAN])
    w_down = din("w_down", [_ne, DE, D])
    lru_wa = din("lru_wa", [NCH, 128, 128])
    lru_wx = din("lru_wx", [NCH, 128, 128])
    wr_d = din("wr", [D, 36])
    rb_d = din("rbias", [128, 36])
    cpar_d = din("cpar", [128, NCH, 12])
    ncol_d = din("ncols", [128, 2, KD])
    nf_d = din("normf", [128, D])
    sa_d = din("sa", [128, NCH, 16, 2])
    sr_d = din("sr", [128, NCH, 16, 3])
    sh_d = din("sh", [128, NCH, 16])
    flag_d = din("flag", [128, 1])
    ident_d = din("ident", [128, 128])
    y_d = dout("y", [2 * N, D])
    ocv_d = dout("o_cv", [128, NCH, 34])
    oxr_d = dout("o_xr", [128, NCH, 51])
    oh_d = dout("o_h", [128, NCH, 17])

    S = Sched(nc, es)

    def sb(name, shape, dt=F32):
        return es.enter_context(nc.sbuf_tensor("sb_" + name, list(shape), dt))

    cpar = sb("cpar", [128, NCH, 12])
    ncols = sb("ncols", [128, 2, KD])
    lwa = sb("lwa", [128, NCH, 128], BF16)
    lwx = sb("lwx", [128, NCH, 128], BF16)
    wr = sb("wrb", [128, KD, 36], BF16)
    rbias = sb("rbias", [128, 36])
    saT = sb("saT", [128, NCH, 16, 2])
    srT = sb("srT", [128, NCH, 16, 3])
    shT = sb("shT", [128, NCH, 16])
    flag = sb("flag", [128, 1])
    ocv = sb("ocv", [128, NCH, 34])
    oxr = sb("oxr", [128, NCH, 51])
    ohh = sb("ohh", [128, NCH, 17])
    ident = sb("ident", [128, 128], BF16)
    cA = sb("cA", [128, NCH])
    cA2 = sb("cA2", [128, NCH])
    hcar = sb("hcar", [128, NCH])
    small = sb("small", [128, 64])
    rt = sb("rt", [128, 256])
    gates = sb("gates", [128, NG, NE])

    B_const = Buf("const")
    B_hcar = Buf("hcar")
    B_small = Buf("small")
    B_rt = Buf("rt")
    B_gates = [Buf("gates%d" % g) for g in range(NG)]
    B_ocv, B_oxr, B_ohh = Buf("ocv"), Buf("oxr"), Buf("ohh")

    ring = sb("ring", [128, RING, GRAN], BF16)
    B_ring = [Buf("ring%d" % i) for i in range(RING)]
    ring_ptr = [0]

    A_WORDS = (120832 + 17408) // 4
    arena = sb("arena", [128, A_WORDS])

    def a32(off_b, n):
        return arena[:, off_b // 4: off_b // 4 + n]

    def a16(off_b, n):
        return arena[:, off_b // 4: off_b // 4 + n // 2].bitcast(BF16)

    O_U = 0
    O_OA = 38912
    O_OR = 38912 + 19456
    O_M = 81920
    O_X = 120832
    uT = a16(O_U, KD * N).rearrange("p (k n) -> p k n", k=KD)
    oaT = a16(O_OA, NCH * N).rearrange("p (k n) -> p k n", k=NCH)
    orT = a16(O_OR, NCH * N).rearrange("p (k n) -> p k n", k=NCH)
    x2 = a32(0, NG * D).rearrange("p (g d) -> p g d", g=NG)
    mT = a16(O_M, KD * N).rearrange("p (k n) -> p k n", k=KD)
    tmpA = [a32(O_M + i * 2432, N) for i in range(16)]
    nfbc = a32(O_M, D)
    xg = [a32(O_OA, D), a32(O_OA + 16384, D)]
    jxL = a16(O_M, D)
    xsL = [a16(O_M + 8192, D), a16(O_M + 16384, D)]
    tmpB = [a32(O_X + i * 2432, N) for i in range(4)]
    hsT = [a16(O_X + i * 4864, 4 * N).rearrange("p (k n) -> p k n", k=4) for i in range(2)]
    slT = [a32(O_X + 9728 + i * 2432, N) for i in range(2)]
    jxX = a16(O_X, D)
    xsX = [a16(O_X, D), a16(O_X + 8192, D)]

    B_uT = [Buf("uT%d" % k) for k in range(KD)]
    B_oa = [Buf("oa%d" % j) for j in range(NCH)]
    B_or = [Buf("or%d" % j) for j in range(NCH)]
    B_x2 = [Buf("x2_%d" % g) for g in range(NG)]
    B_mT = [Buf("mT%d" % k) for k in range(KD)]
    B_tA = [Buf("tA%d" % i) for i in range(16)]
    B_nf = Buf("nf")
    B_xg = [Buf("xg0"), Buf("xg1")]
    B_jxL = Buf("jxL")
    B_xsL = [Buf("xsL0"), Buf("xsL1")]
    B_xsX = [Buf("xsX0"), Buf("xsX1")]
    B_tB = [Buf("tB%d" % i) for i in range(4)]
    B_hs = [Buf("hs0"), Buf("hs1")]
    B_sl = [Buf("sl0"), Buf("sl1")]
    B_jxX = Buf("jxX")

    ps = es.enter_context(nc.psum_tensor("ps", [128, NPS, 512], F32))
    pst = es.enter_context(nc.psum_tensor("pst", [128, 2, 1024], BF16))
    B_ps = [Buf("ps%d" % i) for i in range(NPS)]
    B_pst = [Buf("pst0"), Buf("pst1")]
    ps_ptr = [0]
    pst_ptr = [0]

    def ps_alloc(n):
        p = ps_ptr[0]
        if p + n > NPS:
            p = 0
        ps_ptr[0] = (p + n) % NPS
        return p

    def wload(parts):
        i = ring_ptr[0]
        ring_ptr[0] = (i + 1) % RING
        for pi, (dst_fn, src) in enumerate(parts):
            S.dma("pool", dst_fn(ring[:, i, :]), src, B_ring[i], writes=[B_ring[i]], nodeps=(pi > 0))
        return i

    def v3(k):
        return lambda r: r.rearrange("p (k c) -> p k c", k=k)

    def act(fn, reads, writes):
        return S.op("act", fn, reads, writes)

    def dve(fn, reads, writes):
        return S.op("dve", fn, reads, writes)

    def pe(fn, reads, writes):
        return S.op("pe", fn, reads, writes)

    for dst, src in ((cpar, cpar_d), (ncols, ncol_d), (saT, sa_d), (srT, sr_d), (shT, sh_d), (flag, flag_d)):
        S.dma("sp", dst[:], src, B_const, writes=[B_const])
    S.dma("sp", rbias[:], rb_d, B_const, writes=[B_const])
    B_cp = Buf("constpool")
    S.dma("pool", lwa[:], lru_wa.rearrange("h i o -> i h o"), B_cp, writes=[B_cp])
    S.dma("pool", lwx[:], lru_wx.rearrange("h i o -> i h o"), B_cp, writes=[B_cp], nodeps=True)
    S.dma("pool", wr[:], wr_d.rearrange("(k p) c -> p k c", p=128), B_cp, writes=[B_cp], nodeps=True)

    B_id = B_cp
    S.dma("pool", ident[:], ident_d, B_cp, writes=[B_cp], nodeps=True)

    LAM = 10
    dve(lambda e: e.tensor_copy(small[:, 0:NCH], cpar[:, :, LAM]), [B_const], [B_small])
    act(lambda e: e.activation(small[:, 16:32], small[:, 0:NCH], AF.Exp, scale=-1.0), [B_small], [B_small])
    act(lambda e: e.activation(small[:, 32:48], small[:, 16:32], AF.Ln, bias=1.0), [B_small], [B_small])
    dve(lambda e: e.tensor_scalar(cA[:], small[:, 32:48], -8.0, None, ALU.mult), [B_small], [B_const])
    dve(lambda e: e.tensor_scalar(cA2[:], small[:, 32:48], -16.0, None, ALU.mult), [B_small], [B_const])
    dve(lambda e: e.memset(hcar[:], 0.0), [], [B_hcar])

    B_sm = [Buf("sm%d" % i) for i in range(64)]

    def rms_rstd(src_ap, gs, jx, B_src, B_jx, col):
        bs = B_sm[col]
        dve(lambda e: e.scalar_tensor_tensor(jx[:gs, :], src_ap, 1.0, src_ap, ALU.mult, ALU.mult,
                                             accum_out=small[:gs, col:col + 1]), [B_src], [B_jx, bs])
        dve(lambda e: e.tensor_scalar(small[:gs, col:col + 1], small[:gs, col:col + 1], 1.0 / D, EPS,
                                      ALU.mult, ALU.add), [bs], [bs])
        act(lambda e: e.activation(small[:gs, col:col + 1], small[:gs, col:col + 1], AF.Sqrt),
            [bs], [bs])
        dve(lambda e: e.reciprocal(small[:gs, col:col + 1], small[:gs, col:col + 1]), [bs], [bs])

    def transpose_group(jx, B_jx, gs, s0, dstT, B_dst_list, ncol_idx, single_dst):
        for c0 in range(0, KD, 4):
            h = pst_ptr[0]
            pst_ptr[0] ^= 1

            def tr(e, c0=c0, h=h):
                last = None
                for ci in range(4):
                    last = e.transpose(pst[:, h, ci * 128: ci * 128 + gs],
                                       jx[:gs, (c0 + ci) * 128:(c0 + ci + 1) * 128], ident[:gs, :gs])
                return last
            pe(tr, [B_jx, B_id], [B_pst[h]])
            for ci in range(4):
                k = c0 + ci
                wb = [B_dst_list[0]] if single_dst else [B_dst_list[k]]
                EV = os.environ.get("MK_EV", "both")
                if (h == 0 and EV == "both") or EV == "dve":
                    dve(lambda e, k=k, ci=ci, h=h: e.tensor_scalar(
                        dstT[:, k, s0:s0 + gs], pst[:, h, ci * 128: ci * 128 + gs],
                        ncols[:, ncol_idx, k:k + 1], None, ALU.mult), [B_pst[h], B_const], wb)
                else:
                    act(lambda e, k=k, ci=ci, h=h: e.activation(
                        dstT[:, k, s0:s0 + gs], pst[:, h, ci * 128: ci * 128 + gs],
                        AF.Copy, scale=ncols[:, ncol_idx, k:k + 1]), [B_pst[h], B_const], wb)

    def proj_fm(slot, kn, rhsT, B_rhs, wview=None):
        b = ps_alloc(2)
        wv = ring[:, slot, :].rearrange("p (k c) -> p k c", c=128) if wview is None else wview

        def mm(e, b=b):
            last = None
            for k in range(kn):
                for nt in range(2):
                    last = e.matmul(ps[:, b + nt, 0:NT], wv[:, k, :], rhsT[:, k, nt * NT:(nt + 1) * NT],
                                    start=(k == 0), stop=(k == kn - 1))
            return last
        rd = list(B_rhs) + ([B_ring[slot]] if slot is not None else [B_cp])
        pe(mm, rd, [B_ps[b], B_ps[b + 1]])
        return b

    def psv(b):
        return ps[:, b:b + 2, 0:NT]

    def v2(t):
        return t.rearrange("p (a n) -> p a n", a=2)

    def w_in_chunk(q):
        return wload([(lambda r: r, w_in[q])])

    CW_A, CW_R, CB_R, BA, BX = 0, 3, 7, 8, 9

    def copy_x(bx, XR):
        act(lambda e: e.activation(v2(tmpA[XR]), psv(bx), AF.Copy), [B_ps[bx], B_ps[bx + 1]], [B_tA[XR]])

    def copy_y(by):
        act(lambda e: e.activation(v2(tmpA[8]), psv(by), AF.Copy), [B_ps[by], B_ps[by + 1]], [B_tA[8]])

    def copy_c(bc_):
        act(lambda e: e.activation(v2(tmpA[10]), psv(bc_), AF.Copy), [B_ps[bc_], B_ps[bc_ + 1]], [B_tA[10]])

    def mixer_r(j, P, idx=(0, 1, 2, 3, 4, 5, 6, 7), part=0):
        T = tmpA
        BT = B_tA
        XR, XC, XCB, R, I, Aa, Bt, H = idx
        Y, G = 8, 9
        xr, xc = T[XR], T[XC]
        if part in (0, 1):
            if P["samples"]:
                dve(lambda e: e.tensor_copy(
                    T[XR][:, SB0:SB0 + 176].rearrange("p (b s) -> p b s", s=11)[:, :, 0:3], srT[:, j, :, :]),
                    [B_const], [BT[XR]])
            dve(lambda e: e.memset(xc[:, 0:3], 0.0), [], [BT[XC]])
            act(lambda e: e.activation(xc[:, 3:N], xr[:, 3:N], AF.Identity, bias=cpar[:, j, CB_R:CB_R + 1],
                                       scale=cpar[:, j, CW_R + 3:CW_R + 4]),
                [BT[XR], B_const], [BT[XC]])
            for t in range(3):
                dve(lambda e, t=t: e.scalar_tensor_tensor(
                    xc[:, 3:N], xr[:, t:N - 3 + t], cpar[:, j, CW_R + t:CW_R + t + 1], xc[:, 3:N],
                    ALU.mult, ALU.add), [BT[XR], BT[XC], B_const], [BT[XC]])
            xcb = T[XCB].bitcast(BF16)[:, 0:N]
            act(lambda e: e.activation(xcb, xc, AF.Copy), [BT[XC]], [BT[XCB]])
            xcb3 = xcb.rearrange("p (k n) -> p k n", k=1)
            br = proj_fm(None, 1, xcb3, [BT[XCB]], wview=lwa[:, j:j + 1, :])
            bi = proj_fm(None, 1, xcb3, [BT[XCB]], wview=lwx[:, j:j + 1, :])
            act(lambda e: e.activation(v2(T[R]), psv(br), AF.Sigmoid, bias=cpar[:, j, BA:BA + 1]),
                [B_ps[br], B_ps[br + 1], B_const], [BT[R]])
            act(lambda e: e.activation(v2(T[I]), psv(bi), AF.Sigmoid, bias=cpar[:, j, BX:BX + 1]),
                [B_ps[bi], B_ps[bi + 1], B_const], [BT[I]])
            act(lambda e: e.activation(T[Aa], T[R], AF.Exp, scale=cA[:, j:j + 1]), [BT[R], B_const], [BT[Aa]])
            act(lambda e: e.activation(T[Bt], T[R], AF.Exp, scale=cA2[:, j:j + 1]), [BT[R], B_const], [BT[Bt]])
        if part == 1:
            return
        act(lambda e: e.activation(T[Bt], T[Bt], AF.Relu, bias=1.0, scale=-1.0), [BT[Bt]], [BT[Bt]])
        act(lambda e: e.activation(T[Bt], T[Bt], AF.Sqrt), [BT[Bt]], [BT[Bt]])
        dve(lambda e: e.tensor_tensor(T[Bt], T[Bt], T[I], ALU.mult), [BT[Bt], BT[I]], [BT[Bt]])
        dve(lambda e: e.tensor_tensor(T[Bt], T[Bt], xc, ALU.mult), [BT[Bt], BT[XC]], [BT[Bt]])
        dve(lambda e: e.memset(T[Aa][:, 2:3], 0.0), [], [BT[Aa]])
        if P["init"] == "zero":
            dve(lambda e: e.memset(T[Bt][:, 2:3], 0.0), [], [BT[Bt]])
        else:
            dve(lambda e: e.tensor_copy(T[Bt][:, 2:3], hcar[:, j:j + 1]), [B_hcar], [BT[Bt]])
        if P["samples"]:
            av = T[Aa][:, SB0:SB0 + 176].rearrange("p (b s) -> p b s", s=11)[:, :, 2:3]
            bv = T[Bt][:, SB0:SB0 + 176].rearrange("p (b s) -> p b s", s=11)[:, :, 2:3]
            dve(lambda e: e.memset(av, 0.0), [], [BT[Aa]])
            dve(lambda e: e.tensor_copy(bv, shT[:, j, :].rearrange("p (b o) -> p b o", o=1)),
                [B_const], [BT[Bt]])
        dve(lambda e: e.tensor_tensor_scan(T[H], T[Aa], T[Bt], 0.0, ALU.mult, ALU.add),
            [BT[Aa], BT[Bt]], [BT[H]])
        lc = P["last"]
        if P["carry"] == "flag":
            dve(lambda e: e.tensor_scalar(hcar[:, j:j + 1], T[H][:, lc:lc + 1], flag[:, 0:1], None, ALU.mult),
                [BT[H], B_const], [B_hcar])
        elif P["carry"] == "plain":
            dve(lambda e: e.tensor_copy(hcar[:, j:j + 1], T[H][:, lc:lc + 1]), [BT[H]], [B_hcar])
        if P["samples"]:
            xv = T[XR][:, SB0:SB0 + 176].rearrange("p (b s) -> p b s", s=11)[:, :, 8:11]
            hv = T[H][:, SB0:SB0 + 176].rearrange("p (b s) -> p b s", s=11)[:, :, 10:11]
            dve(lambda e: e.tensor_copy(oxr[:, j, 0:3], T[XR][:, lc - 2:lc + 1]), [BT[XR]], [B_oxr])
            dve(lambda e: e.tensor_copy(oxr[:, j, 3:51].rearrange("p (b s) -> p b s", s=3), xv),
                [BT[XR]], [B_oxr])
            dve(lambda e: e.tensor_copy(ohh[:, j, 0:1], T[H][:, lc:lc + 1]), [BT[H]], [B_ohh])
            dve(lambda e: e.tensor_copy(ohh[:, j, 1:17].rearrange("p (b o) -> p b o", o=1), hv),
                [BT[H]], [B_ohh])
        if not P["main"]:
            return
        dve(lambda e: e.tensor_tensor(T[G], T[Y], T[Y], ALU.mult), [BT[Y]], [BT[G]])
        dve(lambda e: e.tensor_scalar(T[G], T[G], 0.044715, 1.0, ALU.mult, ALU.add), [BT[G]], [BT[G]])
        dve(lambda e: e.tensor_tensor(T[G], T[G], T[Y], ALU.mult), [BT[G], BT[Y]], [BT[G]])
        act(lambda e: e.activation(T[G], T[G], AF.Sigmoid, scale=1.5957691216057308), [BT[G]], [BT[G]])
        dve(lambda e: e.tensor_tensor(T[G], T[G], T[Y], ALU.mult), [BT[G], BT[Y]], [BT[G]])
        dve(lambda e: e.tensor_tensor(orT[:, j, :], T[H], T[G], ALU.mult), [BT[H], BT[G]], [B_or[j]])

    def mixer_a(j, bb_, bv_, P):
        T = tmpA
        BT = B_tA
        C, CV, CA = 10, 11, 12
        dve(lambda e: e.tensor_tensor(v2(T[CV]), v2(T[C]), psv(bv_), ALU.mult),
            [BT[C], B_ps[bv_], B_ps[bv_ + 1]], [BT[CV]])
        cv, ca = T[CV], T[CA]
        if P["samples"]:
            dve(lambda e: e.tensor_copy(
                cv[:, SB0:SB0 + 176].rearrange("p (b s) -> p b s", s=11)[:, :, 1:3], saT[:, j, :, :]),
                [B_const], [BT[CV]])
        dve(lambda e: e.memset(ca[:, 0:2], 0.0), [], [BT[CA]])
        dve(lambda e: e.tensor_scalar(ca[:, 2:N], cv[:, 2:N], cpar[:, j, CW_A + 2:CW_A + 3], None, ALU.mult),
            [BT[CV], B_const], [BT[CA]])
        for t in range(2):
            dve(lambda e, t=t: e.scalar_tensor_tensor(
                ca[:, 2:N], cv[:, t:N - 2 + t], cpar[:, j, CW_A + t:CW_A + t + 1], ca[:, 2:N],
                ALU.mult, ALU.add), [BT[CV], BT[CA], B_const], [BT[CA]])
        dve(lambda e: e.tensor_tensor(v2(oaT[:, j, :]), v2(ca), psv(bb_), ALU.mult),
            [BT[CA], B_ps[bb_], B_ps[bb_ + 1]], [B_oa[j]])
        if P["samples"]:
            lc = P["last"]
            cvv = cv[:, SB0:SB0 + 176].rearrange("p (b s) -> p b s", s=11)[:, :, 9:11]
            dve(lambda e: e.tensor_copy(ocv[:, j, 0:2], cv[:, lc - 1:lc + 1]), [BT[CV]], [B_ocv])
            dve(lambda e: e.tensor_copy(ocv[:, j, 2:34].rearrange("p (b s) -> p b s", s=2), cvv),
                [BT[CV]], [B_ocv])

    OFFS = [0, 128, 256, 384, 512]

    def phase_L(src_d, row0):
        def prep(g):
            gs = GS[g]
            bi = g % 2
            s0 = OFFS[g]
            S.dma("sp", xg[bi][:gs, :], src_d[row0 + s0:row0 + s0 + gs, :], B_xg[bi], writes=[B_xg[bi]])
            rms_rstd(xg[bi][:gs, :], gs, xsL[bi], B_xg[bi], B_xsL[bi], g)
            act(lambda e: e.activation(xsL[bi][:gs, :], xg[bi][:gs, :], AF.Copy, scale=small[:gs, g:g + 1]),
                [B_xg[bi], B_sm[g]], [B_xsL[bi]])
        prep(0)
        for g in range(NG):
            if g + 1 < NG:
                prep(g + 1)
            transpose_group(xsL[g % 2], B_xsL[g % 2], GS[g], OFFS[g], uT, B_uT, 0, False)

    import os
    STOP = int(os.environ.get("MK_STOP", "99"))
    cnt = [0]

    def stop():
        cnt[0] += 1
        return cnt[0] > STOP

    def run_pass(P):
        if stop():
            return
        S.barrier()
        phase_L(P["src"], P["row0"])
        S.barrier()
        if stop():
            return
        if not P["main"]:
            XRS = [0, 8]
            SETS = [tuple(range(0, 8)), tuple(range(8, 16))]

            def proj_x(j):
                sx = w_in_chunk(48 + j)
                bx = proj_fm(sx, KD, uT, B_uT)
                copy_x(bx, XRS[j % 2])
            proj_x(0)
            proj_x(1)
            mixer_r(0, P, idx=SETS[0], part=1)
            for j in range(NCH):
                if j + 2 < NCH:
                    proj_x(j + 2)
                if j + 1 < NCH:
                    mixer_r(j + 1, P, idx=SETS[(j + 1) % 2], part=1)
                mixer_r(j, P, idx=SETS[j % 2], part=2)
            return
        for j in range(NCH):
            sx = w_in_chunk(48 + j)
            bx = proj_fm(sx, KD, uT, B_uT)
            sy = w_in_chunk(64 + j)
            by = proj_fm(sy, KD, uT, B_uT)
            sc = w_in_chunk(16 + j)
            bc_ = proj_fm(sc, KD, uT, B_uT)
            copy_x(bx, 0)
            copy_y(by)
            copy_c(bc_)
            mixer_r(j, P)
            sv = w_in_chunk(32 + j)
            bv_ = proj_fm(sv, KD, uT, B_uT)
            sbb = w_in_chunk(j)
            bb_ = proj_fm(sbb, KD, uT, B_uT)
            mixer_a(j, bb_, bv_, P)
        if not P["main"]:
            return
        if stop():
            return
        S.barrier()
        for m in range(KD):
            sga = w_in_chunk(80 + m)
            bga = proj_fm(sga, KD, uT, B_uT)
            act(lambda e, b=bga: e.activation(v2(tmpB[0]), psv(b), AF.Sigmoid),
                [B_ps[bga], B_ps[bga + 1]], [B_tB[0]])
            sgr = w_in_chunk(112 + m)
            bgr = proj_fm(sgr, KD, uT, B_uT)
            act(lambda e, b=bgr: e.activation(v2(tmpB[1]), psv(b), AF.Sigmoid),
                [B_ps[bgr], B_ps[bgr + 1]], [B_tB[1]])
            sbr = wload([(lambda r: r[:, 0:2048].rearrange("p (k c) -> p k c", c=128),
                          w_bra[:, m * 128:(m + 1) * 128].rearrange("(k p) c -> p k c", p=128)),
                         (lambda r: r[:, 2048:4096].rearrange("p (k c) -> p k c", c=128),
                          w_brr[:, m * 128:(m + 1) * 128].rearrange("(k p) c -> p k c", p=128))])
            wva = ring[:, sbr, 0:2048].rearrange("p (k c) -> p k c", c=128)
            wvr = ring[:, sbr, 2048:4096].rearrange("p (k c) -> p k c", c=128)
            bpa = ps_alloc(2)

            def mma(e, b=bpa, wv=wva):
                last = None
                for k in range(NCH):
                    for nt in range(2):
                        last = e.matmul(ps[:, b + nt, 0:NT], wv[:, k, :], oaT[:, k, nt * NT:(nt + 1) * NT],
                                        start=(k == 0), stop=(k == NCH - 1))
                return last
            pe(mma, B_oa + [B_ring[sbr]], [B_ps[bpa], B_ps[bpa + 1]])
            dve(lambda e, b=bpa: e.tensor_tensor(v2(tmpB[2]), v2(tmpB[0]), psv(b), ALU.mult),
                [B_tB[0], B_ps[bpa], B_ps[bpa + 1]], [B_tB[2]])
            bpr = ps_alloc(2)

            def mmr(e, b=bpr, wv=wvr):
                last = None
                for k in range(NCH):
                    for nt in range(2):
                        last = e.matmul(ps[:, b + nt, 0:NT], wv[:, k, :], orT[:, k, nt * NT:(nt + 1) * NT],
                                        start=(k == 0), stop=(k == NCH - 1))
                return last
            pe(mmr, B_or + [B_ring[sbr]], [B_ps[bpr], B_ps[bpr + 1]])
            dve(lambda e, b=bpr: e.tensor_tensor(v2(tmpB[3]), v2(tmpB[1]), psv(b), ALU.mult),
                [B_tB[1], B_ps[bpr], B_ps[bpr + 1]], [B_tB[3]])
            dve(lambda e, m=m: e.tensor_tensor(mT[:, m, :], tmpB[2], tmpB[3], ALU.add),
                [B_tB[2], B_tB[3]], [B_mT[m]])
        if stop():
            return
        S.barrier()
        s0 = 0
        for g in range(NG):
            gs = GS[g]
            S.dma("sp", x2[:gs, g, :], P["src"][P["row0"] + s0:P["row0"] + s0 + gs, :], B_x2[g],
                  writes=[B_x2[g]])
            s0 += gs
        for blk in range(8):
            slots = []
            for kg in range(4):
                slots.append(wload([(v3(8), w_o[kg * 1024:(kg + 1) * 1024, blk * 512:(blk + 1) * 512]
                                     .rearrange("(k p) c -> p k c", p=128))]))
            banks = [ps_alloc(1) for _ in range(NG)]
            for kg in range(4):
                wv = ring[:, slots[kg], :].rearrange("p (k c) -> p k c", k=8)
                s0 = 0
                for g in range(NG):
                    gs = GS[g]

                    def mmc(e, kg=kg, wv=wv, bank=banks[g], gs=gs, s0=s0):
                        last = None
                        for kk in range(8):
                            k = kg * 8 + kk
                            last = e.matmul(ps[:gs, bank, :], mT[:, k, s0:s0 + gs], wv[:, kk, :],
                                            start=(k == 0), stop=(k == KD - 1))
                        return last
                    pe(mmc, B_mT[kg * 8:(kg + 1) * 8] + [B_ring[slots[kg]]], [B_ps[banks[g]]])
                    s0 += gs
            for g in range(NG):
                gs = GS[g]
                dve(lambda e, g=g, gs=gs, b=banks[g], blk=blk: e.tensor_tensor(
                    x2[:gs, g, blk * 512:(blk + 1) * 512], x2[:gs, g, blk * 512:(blk + 1) * 512],
                    ps[:gs, b, :], ALU.add), [B_x2[g], B_ps[banks[g]]], [B_x2[g]])
        if stop():
            return
        S.barrier()
        def prep_n(g):
            gs = GS[g]
            bi = g % 2
            rms_rstd(x2[:gs, g, :], gs, xsX[bi], B_x2[g], B_xsX[bi], 8 + g)
            act(lambda e: e.activation(xsX[bi][:gs, :], x2[:gs, g, :], AF.Copy, scale=small[:gs, 8 + g:9 + g]),
                [B_x2[g], B_sm[8 + g]], [B_xsX[bi]])
        prep_n(0)
        for g in range(NG):
            gs = GS[g]
            s0 = OFFS[g]
            if g + 1 < NG:
                prep_n(g + 1)
            transpose_group(xsX[g % 2], B_xsX[g % 2], gs, s0, mT, B_mT, 1, False)
            bl = ps_alloc(1)

            def mmr2(e, gs=gs, s0=s0, bl=bl):
                last = None
                for k in range(KD):
                    last = e.matmul(ps[:gs, bl, 0:36], mT[:, k, s0:s0 + gs], wr[:, k, :],
                                    start=(k == 0), stop=(k == KD - 1))
                return last
            pe(mmr2, B_mT + [B_cp], [B_ps[bl]])
            routing(g, gs, bl)
        if stop():
            return
        S.barrier()
        for ex in range(NE):
            hb = ex % 2
            for hc in range(4):
                sg = wload([(lambda r: r, w_gate[ex, hc])])
                bg = proj_fm(sg, KD, mT, B_mT)
                act(lambda e, b=bg, hc=hc: e.activation(v2(slT[hc % 2]), psv(b), AF.Silu),
                    [B_ps[bg], B_ps[bg + 1]], [B_sl[hc % 2]])
                su = wload([(lambda r: r, w_up[ex, hc])])
                bu = proj_fm(su, KD, mT, B_mT)
                dve(lambda e, b=bu, hc=hc, hb=hb: e.tensor_tensor(
                    v2(hsT[hb][:, hc, :]), v2(slT[hc % 2]), psv(b), ALU.mult),
                    [B_sl[hc % 2], B_ps[bu], B_ps[bu + 1]], [B_hs[hb]])
            for b2 in range(4):
                sd = wload([(v3(4), w_down[ex, :, b2 * 1024:(b2 + 1) * 1024].rearrange("(k p) c -> p k c", p=128))])
                wv = ring[:, sd, :].rearrange("p (k c) -> p k c", k=4)
                s0 = 0
                for g in range(NG):
                    gs = GS[g]
                    b = ps_alloc(2)

                    def mmd(e, wv=wv, b=b, hb=hb, gs=gs, s0=s0):
                        last = None
                        for k in range(4):
                            for half in range(2):
                                last = e.matmul(ps[:gs, b + half, :], hsT[hb][:, k, s0:s0 + gs],
                                                wv[:, k, half * 512:(half + 1) * 512],
                                                start=(k == 0), stop=(k == 3))
                        return last
                    pe(mmd, [B_hs[hb], B_ring[sd]], [B_ps[b], B_ps[b + 1]])
                    dve(lambda e, g=g, gs=gs, b=b, b2=b2, ex=ex: e.scalar_tensor_tensor(
                        x2[:gs, g, b2 * 1024:(b2 + 1) * 1024].rearrange("p (a c) -> p a c", a=2),
                        ps[:gs, b:b + 2, :], gates[:gs, g, ex:ex + 1],
                        x2[:gs, g, b2 * 1024:(b2 + 1) * 1024].rearrange("p (a c) -> p a c", a=2),
                        ALU.mult, ALU.add),
                        [B_x2[g], B_ps[b], B_ps[b + 1], B_gates[g]], [B_x2[g]])
                    s0 += gs
        if stop():
            return
        S.barrier()
        S.dma("sp", nfbc, nf_d, B_nf, writes=[B_nf])
        s0 = 0
        for g in range(NG):
            gs = GS[g]
            rms_rstd(x2[:gs, g, :], gs, jxX, B_x2[g], B_jxX, 16 + g)
            dve(lambda e, gs=gs, g=g: e.scalar_tensor_tensor(
                x2[:gs, g, :], x2[:gs, g, :], small[:gs, 16 + g:17 + g], nfbc[:gs, :], ALU.mult, ALU.mult),
                [B_x2[g], B_sm[16 + g], B_nf], [B_x2[g]])
            S.dma("sp", y_d[P["row0"] + s0:P["row0"] + s0 + gs, :], x2[:gs, g, :], B_x2[g], reads=[B_x2[g]])
            s0 += gs

    def routing(g, gs, bl):
        R_ = rt
        lg = R_[:gs, 0:36]
        gl = R_[:gs, 0:4]
        rd = [B_rt]
        wrt = [B_rt]
        dve(lambda e: e.tensor_tensor(lg, ps[:gs, bl, 0:36], rbias[:gs, :], ALU.add), [B_ps[bl], B_const], wrt)
        gmax = R_[:gs, 40:41]
        dve(lambda e: e.tensor_reduce(gmax, gl, AX.X, ALU.max), rd, wrt)
        ngm = R_[:gs, 41:42]
        dve(lambda e: e.tensor_scalar(ngm, gmax, -1.0, None, ALU.mult), rd, wrt)
        ge = R_[:gs, 44:48]
        act(lambda e: e.activation(ge, gl, AF.Exp, bias=ngm, scale=1.0), rd, wrt)
        gsum = R_[:gs, 42:43]
        dve(lambda e: e.tensor_reduce(gsum, ge, AX.X, ALU.add), rd, wrt)
        gp = R_[:gs, 43:44]
        dve(lambda e: e.reciprocal(gp, gsum), rd, wrt)
        ohg = R_[:gs, 48:52]
        dve(lambda e: e.tensor_scalar(ohg, gl, gmax, None, ALU.is_equal), rd, wrt)
        esel = R_[:gs, 56:64]
        dve(lambda e: e.tensor_scalar(esel, R_[:gs, 4:12], ohg[:, 0:1], None, ALU.mult), rd, wrt)
        for q in range(1, 4):
            dve(lambda e, q=q: e.scalar_tensor_tensor(esel, R_[:gs, 4 + 8 * q:12 + 8 * q], ohg[:, q:q + 1], esel,
                                                      ALU.mult, ALU.add), rd, wrt)
        m1 = R_[:gs, 64:65]
        dve(lambda e: e.tensor_reduce(m1, esel, AX.X, ALU.max), rd, wrt)
        oh1 = R_[:gs, 72:80]
        dve(lambda e: e.tensor_scalar(oh1, esel, m1, None, ALU.is_equal), rd, wrt)
        e2 = R_[:gs, 80:88]
        dve(lambda e: e.scalar_tensor_tensor(e2, oh1, -1.0e30, esel, ALU.mult, ALU.add), rd, wrt)
        m2 = R_[:gs, 65:66]
        dve(lambda e: e.tensor_reduce(m2, e2, AX.X, ALU.max), rd, wrt)
        oh2 = R_[:gs, 88:96]
        dve(lambda e: e.tensor_scalar(oh2, e2, m2, None, ALU.is_equal), rd, wrt)
        dd = R_[:gs, 66:67]
        dve(lambda e: e.tensor_tensor(dd, m2, m1, ALU.subtract), rd, wrt)
        ed = R_[:gs, 67:68]
        act(lambda e: e.activation(ed, dd, AF.Exp), rd, wrt)
        den = R_[:gs, 68:69]
        dve(lambda e: e.tensor_scalar(den, ed, 1.0, None, ALU.add), rd, wrt)
        rden = R_[:gs, 69:70]
        dve(lambda e: e.reciprocal(rden, den), rd, wrt)
        w1 = R_[:gs, 70:71]
        dve(lambda e: e.tensor_tensor(w1, rden, gp, ALU.mult), rd, wrt)
        w2 = R_[:gs, 71:72]
        dve(lambda e: e.tensor_tensor(w2, w1, ed, ALU.mult), rd, wrt)
        g8 = R_[:gs, 96:104]
        dve(lambda e: e.tensor_scalar(g8, oh1, w1, None, ALU.mult), rd, wrt)
        dve(lambda e: e.scalar_tensor_tensor(g8, oh2, w2, g8, ALU.mult, ALU.add), rd, wrt)
        for q in range(4):
            dve(lambda e, q=q: e.tensor_scalar(gates[:gs, g, 8 * q:8 * q + 8], g8, ohg[:, q:q + 1], None, ALU.mult),
                rd, [B_gates[g]])

    run_pass(dict(src=xpre, row0=0, main=False, samples=False, init="zero", carry="plain", last=N - 1))
    run_pass(dict(src=xpre, row0=N, main=False, samples=False, init="carry", carry="flag", last=SB0 - 1))
    run_pass(dict(src=xin, row0=0, main=True, samples=False, init="carry", carry="plain", last=N - 1))
    run_pass(dict(src=xin, row0=N, main=True, samples=True, init="carry", carry="none", last=SB0 - 1))
    if cnt[0] <= STOP:
        S.dma("sp", ocv_d, ocv[:], B_ocv, reads=[B_ocv])
        S.dma("sp", oxr_d, oxr[:], B_oxr, reads=[B_oxr])
        S.dma("sp", oh_d, ohh[:], B_ohh, reads=[B_ohh])
    sp = S.engs["sp"]
    for ev in S.pending_dma:
        S._wait(sp, ev)
    S.barrier()

    with nc.Block() as block:
        @block.tensor
        def _(t):
            S.replay("pe", t)

        @block.scalar
        def _(a):
            S.replay("act", a)

        @block.vector
        def _(v):
            S.replay("dve", v)

        @block.sync
        def _(s):
            S.replay("sp", s)

        @block.gpsimd
        def _(g):
            S.replay("pool", g)


_NC_CACHE = {}


def _get_nc():
    if "nc" not in _NC_CACHE:
        _NC_CACHE["nc"] = build_program()
    return _NC_CACHE["nc"]


def _chunked(v, nchunk):
    return np.ascontiguousarray(np.moveaxis(v.reshape(v.shape[:-1] + (nchunk, 128)), -1, 0))


def _gu_layout(w):
    e = w.shape[0]
    return np.ascontiguousarray(w.reshape(e, KD, 128, 4, 128).transpose(0, 3, 2, 1, 4)).reshape(e, 4, 128, GRAN)


def kernel(x_prompt, x_sample, state_conv_a, state_conv_r, state_h, meta_tokens, norm1, w_in, conv_a_w, conv_r_w,
           conv_r_b, lru_wa, lru_ba, lru_wx, lru_bx, lru_lam, w_br_a, w_br_r, w_o, norm2, w_group, b_group,
           w_router, b_router, w_gate, w_up, w_down, norm_f):
    f = np.float32
    x_prompt = np.asarray(x_prompt, f)
    x_sample = np.asarray(x_sample, f)
    H = 1032
    cp = np.zeros((12, W), f)
    cp[0:3] = np.asarray(conv_a_w, f)[0]
    cp[3:7] = np.asarray(conv_r_w, f)[0]
    cp[7] = np.asarray(conv_r_b, f)[0]
    cp[8] = np.asarray(lru_ba, f)[0]
    cp[9] = np.asarray(lru_bx, f)[0]
    cp[10] = np.asarray(lru_lam, f)[0]
    cpar = np.ascontiguousarray(cp.reshape(12, NCH, 128).transpose(2, 1, 0))
    ncols = np.ascontiguousarray(
        np.stack([np.asarray(norm1, f)[0], np.asarray(norm2, f)[0]]).reshape(2, KD, 128).transpose(2, 0, 1))
    wr = np.ascontiguousarray(np.concatenate([np.asarray(w_group, f)[0], np.asarray(w_router, f)[0]], axis=1))
    rbias = np.ascontiguousarray(np.broadcast_to(np.concatenate([np.asarray(b_group, f)[0], np.asarray(b_router, f)[0]])[None, :], (128, 36)))
    shared = {
        "w_in": np.ascontiguousarray(np.asarray(w_in, f)[0].reshape(KD, 128, 144, 128).transpose(2, 1, 0, 3))
        .reshape(144, 128, GRAN), "w_br_a": np.asarray(w_br_a, f)[0], "w_br_r": np.asarray(w_br_r, f)[0],
        "w_o": np.asarray(w_o, f)[0], "w_gate": _gu_layout(np.asarray(w_gate, f)[0]), "w_up": _gu_layout(np.asarray(w_up, f)[0]),
        "w_down": np.asarray(w_down, f)[0], "lru_wa": np.asarray(lru_wa, f)[0], "lru_wx": np.asarray(lru_wx, f)[0],
        "wr": wr, "rbias": rbias, "cpar": cpar, "ncols": ncols, "normf": np.ascontiguousarray(np.broadcast_to(np.asarray(norm_f, f)[None, :], (128, D))),
        "ident": np.eye(128, dtype=f),
    }
    meta = np.asarray(meta_tokens, f)
    sca = np.asarray(state_conv_a, f)[0]
    scr = np.asarray(state_conv_r, f)[0]
    sth = np.asarray(state_h, f)[0]
    in_maps = []
    for c in range(NCORES):
        s, half = c // 2, c % 2
        full = np.concatenate([meta, x_prompt[s]], axis=0)
        xpre = np.zeros((2 * N, D), f)
        xin = np.zeros((2 * N, D), f)
        if half == 1:
            xpre[3:N] = full[0:605]
            xpre[N:N + 3] = full[602:605]
            xpre[N + 3:N + SB0] = full[605:H]
            xin[0:3] = full[H - 3:H]
        base = half * H
        xin[3:N] = full[base:base + 605]
        xin[N:N + 3] = full[base + 602:base + 605]
        xin[N + 3:N + SB0] = full[base + 605:base + H]
        for bl in range(16):
            r0 = N + SB0 + 11 * bl + 3
            xin[r0:r0 + 8] = x_sample[16 * c + bl]
        sa = sca[16 * c:16 * c + 16]
        sr = scr[16 * c:16 * c + 16]
        sh = sth[16 * c:16 * c + 16]
        m = dict(shared)
        m.update({
            "xpre": xpre, "xin": xin,
            "sa": np.ascontiguousarray(sa.reshape(16, 2, NCH, 128).transpose(3, 2, 0, 1)),
            "sr": np.ascontiguousarray(sr.reshape(16, 3, NCH, 128).transpose(3, 2, 0, 1)),
            "sh": np.ascontiguousarray(sh.reshape(16, NCH, 128).transpose(2, 1, 0)),
            "flag": np.full((128, 1), float(half), f),
        })
        in_maps.append(m)
    import os
    if os.environ.get("MK_TINY"):
        for m in in_maps:
            for k in ("w_br_a", "w_br_r", "w_o"):
                m[k] = m[k][:128]
    if os.environ.get("MK_SMALL"):
        for m in in_maps:
            for k in ("w_gate", "w_up", "w_down"):
                m[k] = m[k][0:1]
    nc = _get_nc()
    res = run_bass_kernel_spmd(nc, in_maps, core_ids=list(range(NCORES)))
    R = res.results
    B, SEQ = x_prompt.shape[0], x_prompt.shape[1]
    y_prompt = np.zeros((B, SEQ, D), f)
    y_sample = np.zeros((x_sample.shape[0], 8, D), f)
    pa = np.zeros((1, B, 2, W), f)
    pr = np.zeros((1, B, 3, W), f)
    ph = np.zeros((1, B, W), f)
    sa_o = np.zeros((1, 128, 2, W), f)
    sr_o = np.zeros((1, 128, 3, W), f)
    sh_o = np.zeros((1, 128, W), f)

    def unchunk(a):
        return a.transpose(2, 1, 0).reshape(a.shape[2], W)
    for c in range(NCORES):
        s, half = c // 2, c % 2
        y = R[c]["y"]
        rows = np.concatenate([y[3:N], y[N + 3:N + SB0]], axis=0)
        if half == 0:
            y_prompt[s, 0:H - 16] = rows[16:]
        else:
            y_prompt[s, H - 16:] = rows
        ys = y[N + SB0:N + SB0 + 176].reshape(16, 11, D)[:, 3:, :]
        y_sample[16 * c:16 * c + 16] = ys
        cv = unchunk(R[c]["o_cv"])
        xr = unchunk(R[c]["o_xr"])
        hh = unchunk(R[c]["o_h"])
        if half == 1:
            pa[0, s] = cv[0:2]
            pr[0, s] = xr[0:3]
            ph[0, s] = hh[0]
        sa_o[0, 16 * c:16 * c + 16] = cv[2:].reshape(16, 2, W)
        sr_o[0, 16 * c:16 * c + 16] = xr[3:].reshape(16, 3, W)
        sh_o[0, 16 * c:16 * c + 16] = hh[1:]
    return (y_prompt, y_sample, pa, pr, ph, sa_o, sr_o, sh_o)
```

```python
import contextlib
import numpy as np
import concourse.bass as bass
import concourse.mybir as mybir
from concourse.bass_utils import run_bass_kernel_spmd

F32 = mybir.dt.float32
BF16 = mybir.dt.bfloat16
AF = mybir.ActivationFunctionType
ALU = mybir.AluOpType
AX = mybir.AxisListType

D = 4096
W = 2048
NCH = 16
KD = 32
N = 608
NT = 304
GS = [128, 128, 128, 128, 96]
NG = 5
NE = 32
DE = 512
EPS = 1e-6
SB0 = 430
NCORES = 8
RING = 4
NPS = 6
GRAN = 4096


class Buf:
    __slots__ = ("lw", "rd", "dsem", "dcnt", "name")

    def __init__(self, name=""):
        self.lw = None
        self.rd = []
        self.dsem = None
        self.dcnt = 0
        self.name = name


class Eng:
    def __init__(self, name, sem, is_pe=False, is_dma=False):
        self.name = name
        self.sem = sem
        self.seq = 0
        self.items = []
        self.waited = {}
        self.is_pe = is_pe
        self.is_dma = is_dma


class Sched:
    def __init__(self, nc, es):
        self.nc = nc
        self.es = es
        self.nsem = 0
        self.engs = {}
        for nm in ("pe", "act", "dve", "sp", "pool"):
            self.engs[nm] = Eng(nm, self.newsem(nm), is_pe=(nm == "pe"), is_dma=nm in ("sp", "pool"))
        self.pending_dma = []

    def newsem(self, name):
        self.nsem += 1
        return self.es.enter_context(self.nc.semaphore("s_%s_%d" % (name, self.nsem)))

    def _wait(self, eng, ev):
        key, sem, val = ev
        if eng.waited.get(key, 0) >= val:
            return
        eng.waited[key] = val
        eng.items.append(("w", sem, val))

    def _deps(self, eng, reads, writes):
        for b in reads:
            if b.lw is not None:
                self._dep(eng, b.lw, raw=True)
        for b in writes:
            if b.lw is not None:
                self._dep(eng, b.lw, raw=True)
            for ev in b.rd:
                self._dep(eng, ev, raw=False)

    def _dep(self, eng, ev, raw):
        key = ev[0]
        if key == eng.name:
            if eng.is_pe:
                return
        self._wait(eng, ev)

    def op(self, engname, fn, reads=(), writes=()):
        eng = self.engs[engname]
        self._deps(eng, reads, writes)
        eng.seq += 1
        ev = (eng.name, eng.sem, eng.seq)
        eng.items.append(("o", fn, eng.sem, 1))
        for b in reads:
            b.rd.append(ev)
        for b in writes:
            b.lw = ev
            b.rd = []
        return ev

    def dma(self, engname, out, in_, owner, reads=(), writes=(), nodeps=False):
        eng = self.engs[engname]
        if owner.dsem is None:
            owner.dsem = self.newsem("d")
        if not nodeps:
            self._deps(eng, reads, writes)
        owner.dcnt += 16
        ev = ("d%d" % id(owner), owner.dsem, owner.dcnt)
        eng.items.append(("o", lambda e, o=out, i=in_: e.dma_start(out=o, in_=i), owner.dsem, 16))
        for b in reads:
            b.rd.append(ev)
        for b in writes:
            b.lw = ev
            b.rd = []
        if engname == "sp":
            self.pending_dma.append(ev)
        return ev

    def barrier(self):
        for nm in ("pe", "act", "dve", "sp"):
            e = self.engs[nm]
            for nm2 in ("pe", "act", "dve"):
                e2 = self.engs[nm2]
                if nm2 != nm and e2.seq > 0:
                    self._wait(e, (e2.name, e2.sem, e2.seq))
            for ev in self.pending_dma:
                self._wait(e, ev)
        self.pending_dma = []

    def replay(self, engname, h):
        for it in self.engs[engname].items:
            if it[0] == "w":
                h.wait_ge(it[1], it[2])
            else:
                ins = it[1](h)
                ins.then_inc(it[2], it[3])


def build_program():
    nc = bass.Bass("TRN2", target_bir_lowering=False)
    es = contextlib.ExitStack()
    with es:
        _build(nc, es)
    return nc


def _build(nc, es):
    def din(name, shape):
        return nc.dram_tensor(name, list(shape), F32, kind="ExternalInput").ap()

    def dout(name, shape):
        return nc.dram_tensor(name, list(shape), F32, kind="ExternalOutput").ap()

    xpre = din("xpre", [2 * N, D])
    xin = din("xin", [2 * N, D])
    import os
    _tiny = bool(os.environ.get("MK_TINY"))
    w_in = din("w_in", [144, 128, GRAN])
    w_bra = din("w_br_a", [128 if _tiny else W, D])
    w_brr = din("w_br_r", [128 if _tiny else W, D])
    w_o = din("w_o", [128 if _tiny else D, D])
    import os
    _ne = 1 if os.environ.get("MK_SMALL") else NE
    w_gate = din("w_gate", [_ne, 4, 128, GRAN])
    w_up = din("w_up", [_ne, 4, 128, GRAN])
    w_down = din("w_down", [_ne, DE, D])
    lru_wa = din("lru_wa", [NCH, 128, 128])
    lru_wx = din("lru_wx", [NCH, 128, 128])
    wr_d = din("wr", [D, 36])
    rb_d = din("rbias", [128, 36])
    cpar_d = din("cpar", [128, NCH, 12])
    ncol_d = din("ncols", [128, 2, KD])
    nf_d = din("normf", [128, D])
    sa_d = din("sa", [128, NCH, 16, 2])
    sr_d = din("sr", [128, NCH, 16, 3])
    sh_d = din("sh", [128, NCH, 16])
    flag_d = din("flag", [128, 1])
    ident_d = din("ident", [128, 128])
    y_d = dout("y", [2 * N, D])
    ocv_d = dout("o_cv", [128, NCH, 34])
    oxr_d = dout("o_xr", [128, NCH, 51])
    oh_d = dout("o_h", [128, NCH, 17])

    S = Sched(nc, es)

    def sb(name, shape, dt=F32):
        return es.enter_context(nc.sbuf_tensor("sb_" + name, list(shape), dt))

    cpar = sb("cpar", [128, NCH, 12])
    ncols = sb("ncols", [128, 2, KD])
    lwa = sb("lwa", [128, NCH, 128], BF16)
    lwx = sb("lwx", [128, NCH, 128], BF16)
    wr = sb("wrb", [128, KD, 36], BF16)
    rbias = sb("rbias", [128, 36])
    saT = sb("saT", [128, NCH, 16, 2])
    srT = sb("srT", [128, NCH, 16, 3])
    shT = sb("shT", [128, NCH, 16])
    flag = sb("flag", [128, 1])
    ocv = sb("ocv", [128, NCH, 34])
    oxr = sb("oxr", [128, NCH, 51])
    ohh = sb("ohh", [128, NCH, 17])
    ident = sb("ident", [128, 128], BF16)
    cA = sb("cA", [128, NCH])
    cA2 = sb("cA2", [128, NCH])
    hcar = sb("hcar", [128, NCH])
    small = sb("small", [128, 64])
    rt = sb("rt", [128, 256])
    gates = sb("gates", [128, NG, NE])

    B_const = Buf("const")
    B_hcar = Buf("hcar")
    B_small = Buf("small")
    B_rt = Buf("rt")
    B_gates = [Buf("gates%d" % g) for g in range(NG)]
    B_ocv, B_oxr, B_ohh = Buf("ocv"), Buf("oxr"), Buf("ohh")

    ring = sb("ring", [128, RING, GRAN], BF16)
    B_ring = [Buf("ring%d" % i) for i in range(RING)]
    ring_ptr = [0]

    A_WORDS = (120832 + 17408) // 4
    arena = sb("arena", [128, A_WORDS])

    def a32(off_b, n):
        return arena[:, off_b // 4: off_b // 4 + n]

    def a16(off_b, n):
        return arena[:, off_b // 4: off_b // 4 + n // 2].bitcast(BF16)

    O_U = 0
    O_OA = 38912
    O_OR = 38912 + 19456
    O_M = 81920
    O_X = 120832
    uT = a16(O_U, KD * N).rearrange("p (k n) -> p k n", k=KD)
    oaT = a16(O_OA, NCH * N).rearrange("p (k n) -> p k n", k=NCH)
    orT = a16(O_OR, NCH * N).rearrange("p (k n) -> p k n", k=NCH)
    x2 = a32(0, NG * D).rearrange("p (g d) -> p g d", g=NG)
    mT = a16(O_M, KD * N).rearrange("p (k n) -> p k n", k=KD)
    tmpA = [a32(O_M + i * 2432, N) for i in range(16)]
    nfbc = a32(O_M, D)
    xg = [a32(O_OA, D), a32(O_OA + 16384, D)]
    jxL = a16(O_M, D)
    xsL = [a16(O_M + 8192, D), a16(O_M + 16384, D)]
    tmpB = [a32(O_X + i * 2432, N) for i in range(4)]
    hsT = [a16(O_X + i * 4864, 4 * N).rearrange("p (k n) -> p k n", k=4) for i in range(2)]
    slT = [a32(O_X + 9728 + i * 2432, N) for i in range(2)]
    jxX = a16(O_X, D)
    xsX = [a16(O_X, D), a16(O_X + 8192, D)]

    B_uT = [Buf("uT%d" % k) for k in range(KD)]
    B_oa = [Buf("oa%d" % j) for j in range(NCH)]
    B_or = [Buf("or%d" % j) for j in range(NCH)]
    B_x2 = [Buf("x2_%d" % g) for g in range(NG)]
    B_mT = [Buf("mT%d" % k) for k in range(KD)]
    B_tA = [Buf("tA%d" % i) for i in range(16)]
    B_nf = Buf("nf")
    B_xg = [Buf("xg0"), Buf("xg1")]
    B_jxL = Buf("jxL")
    B_xsL = [Buf("xsL0"), Buf("xsL1")]
    B_xsX = [Buf("xsX0"), Buf("xsX1")]
    B_tB = [Buf("tB%d" % i) for i in range(4)]
    B_hs = [Buf("hs0"), Buf("hs1")]
    B_sl = [Buf("sl0"), Buf("sl1")]
    B_jxX = Buf("jxX")

    ps = es.enter_context(nc.psum_tensor("ps", [128, NPS, 512], F32))
    pst = es.enter_context(nc.psum_tensor("pst", [128, 2, 1024], BF16))
    B_ps = [Buf("ps%d" % i) for i in range(NPS)]
    B_pst = [Buf("pst0"), Buf("pst1")]
    ps_ptr = [0]
    pst_ptr = [0]

    def ps_alloc(n):
        p = ps_ptr[0]
        if p + n > NPS:
            p = 0
        ps_ptr[0] = (p + n) % NPS
        return p

    def wload(parts):
        i = ring_ptr[0]
        ring_ptr[0] = (i + 1) % RING
        for pi, (dst_fn, src) in enumerate(parts):
            S.dma("pool", dst_fn(ring[:, i, :]), src, B_ring[i], writes=[B_ring[i]], nodeps=(pi > 0))
        return i

    def v3(k):
        return lambda r: r.rearrange("p (k c) -> p k c", k=k)

    def act(fn, reads, writes):
        return S.op("act", fn, reads, writes)

    def dve(fn, reads, writes):
        return S.op("dve", fn, reads, writes)

    def pe(fn, reads, writes):
        return S.op("pe", fn, reads, writes)

    for dst, src in ((cpar, cpar_d), (ncols, ncol_d), (saT, sa_d), (srT, sr_d), (shT, sh_d), (flag, flag_d)):
        S.dma("sp", dst[:], src, B_const, writes=[B_const])
    S.dma("sp", rbias[:], rb_d, B_const, writes=[B_const])
    B_cp = Buf("constpool")
    S.dma("pool", lwa[:], lru_wa.rearrange("h i o -> i h o"), B_cp, writes=[B_cp])
    S.dma("pool", lwx[:], lru_wx.rearrange("h i o -> i h o"), B_cp, writes=[B_cp], nodeps=True)
    S.dma("pool", wr[:], wr_d.rearrange("(k p) c -> p k c", p=128), B_cp, writes=[B_cp], nodeps=True)

    B_id = B_cp
    S.dma("pool", ident[:], ident_d, B_cp, writes=[B_cp], nodeps=True)

    LAM = 10
    dve(lambda e: e.tensor_copy(small[:, 0:NCH], cpar[:, :, LAM]), [B_const], [B_small])
    act(lambda e: e.activation(small[:, 16:32], small[:, 0:NCH], AF.Exp, scale=-1.0), [B_small], [B_small])
    act(lambda e: e.activation(small[:, 32:48], small[:, 16:32], AF.Ln, bias=1.0), [B_small], [B_small])
    dve(lambda e: e.tensor_scalar(cA[:], small[:, 32:48], -8.0, None, ALU.mult), [B_small], [B_const])
    dve(lambda e: e.tensor_scalar(cA2[:], small[:, 32:48], -16.0, None, ALU.mult), [B_small], [B_const])
    dve(lambda e: e.memset(hcar[:], 0.0), [], [B_hcar])

    B_sm = [Buf("sm%d" % i) for i in range(64)]

    def rms_rstd(src_ap, gs, jx, B_src, B_jx, col):
        bs = B_sm[col]
        dve(lambda e: e.scalar_tensor_tensor(jx[:gs, :], src_ap, 1.0, src_ap, ALU.mult, ALU.mult,
                                             accum_out=small[:gs, col:col + 1]), [B_src], [B_jx, bs])
        dve(lambda e: e.tensor_scalar(small[:gs, col:col + 1], small[:gs, col:col + 1], 1.0 / D, EPS,
                                      ALU.mult, ALU.add), [bs], [bs])
        act(lambda e: e.activation(small[:gs, col:col + 1], small[:gs, col:col + 1], AF.Sqrt),
            [bs], [bs])
        dve(lambda e: e.reciprocal(small[:gs, col:col + 1], small[:gs, col:col + 1]), [bs], [bs])

    def transpose_group(jx, B_jx, gs, s0, dstT, B_dst_list, ncol_idx, single_dst):
        for c0 in range(0, KD, 4):
            h = pst_ptr[0]
            pst_ptr[0] ^= 1

            def tr(e, c0=c0, h=h):
                last = None
                for ci in range(4):
                    last = e.transpose(pst[:, h, ci * 128: ci * 128 + gs],
                                       jx[:gs, (c0 + ci) * 128:(c0 + ci + 1) * 128], ident[:gs, :gs])
                return last
            pe(tr, [B_jx, B_id], [B_pst[h]])
            for ci in range(4):
                k = c0 + ci
                wb = [B_dst_list[0]] if single_dst else [B_dst_list[k]]
                EV = os.environ.get("MK_EV", "both")
                if (h == 0 and EV == "both") or EV == "dve":
                    dve(lambda e, k=k, ci=ci, h=h: e.tensor_scalar(
                        dstT[:, k, s0:s0 + gs], pst[:, h, ci * 128: ci * 128 + gs],
                        ncols[:, ncol_idx, k:k + 1], None, ALU.mult), [B_pst[h], B_const], wb)
                else:
                    act(lambda e, k=k, ci=ci, h=h: e.activation(
                        dstT[:, k, s0:s0 + gs], pst[:, h, ci * 128: ci * 128 + gs],
                        AF.Copy, scale=ncols[:, ncol_idx, k:k + 1]), [B_pst[h], B_const], wb)

    def proj_fm(slot, kn, rhsT, B_rhs, wview=None):
        b = ps_alloc(2)
        wv = ring[:, slot, :].rearrange("p (k c) -> p k c", c=128) if wview is None else wview

        def mm(e, b=b):
            last = None
            for k in range(kn):
                for nt in range(2):
                    last = e.matmul(ps[:, b + nt, 0:NT], wv[:, k, :], rhsT[:, k, nt * NT:(nt + 1) * NT],
                                    start=(k == 0), stop=(k == kn - 1))
            return last
        rd = list(B_rhs) + ([B_ring[slot]] if slot is not None else [B_cp])
        pe(mm, rd, [B_ps[b], B_ps[b + 1]])
        return b

    def psv(b):
        return ps[:, b:b + 2, 0:NT]

    def v2(t):
        return t.rearrange("p (a n) -> p a n", a=2)

    def w_in_chunk(q):
        return wload([(lambda r: r, w_in[q])])

    CW_A, CW_R, CB_R, BA, BX = 0, 3, 7, 8, 9

    def copy_x(bx, XR):
        act(lambda e: e.activation(v2(tmpA[XR]), psv(bx), AF.Copy), [B_ps[bx], B_ps[bx + 1]], [B_tA[XR]])

    def copy_y(by):
        act(lambda e: e.activation(v2(tmpA[8]), psv(by), AF.Copy), [B_ps[by], B_ps[by + 1]], [B_tA[8]])

    def copy_c(bc_):
        act(lambda e: e.activation(v2(tmpA[10]), psv(bc_), AF.Copy), [B_ps[bc_], B_ps[bc_ + 1]], [B_tA[10]])

    def mixer_r(j, P, idx=(0, 1, 2, 3, 4, 5, 6, 7), part=0):
        T = tmpA
        BT = B_tA
        XR, XC, XCB, R, I, Aa, Bt, H = idx
        Y, G = 8, 9
        xr, xc = T[XR], T[XC]
        if part in (0, 1):
            if P["samples"]:
                dve(lambda e: e.tensor_copy(
                    T[XR][:, SB0:SB0 + 176].rearrange("p (b s) -> p b s", s=11)[:, :, 0:3], srT[:, j, :, :]),
                    [B_const], [BT[XR]])
            dve(lambda e: e.memset(xc[:, 0:3], 0.0), [], [BT[XC]])
            dve(lambda e: e.tensor_scalar(xc[:, 3:N], xr[:, 3:N], cpar[:, j, CW_R + 3:CW_R + 4],
                                          cpar[:, j, CB_R:CB_R + 1], ALU.mult, ALU.add),
                [BT[XR], B_const], [BT[XC]])
            for t in range(3):
                dve(lambda e, t=t: e.scalar_tensor_tensor(
                    xc[:, 3:N], xr[:, t:N - 3 + t], cpar[:, j, CW_R + t:CW_R + t + 1], xc[:, 3:N],
                    ALU.mult, ALU.add), [BT[XR], BT[XC], B_const], [BT[XC]])
            xcb = T[XCB].bitcast(BF16)[:, 0:N]
            act(lambda e: e.activation(xcb, xc, AF.Copy), [BT[XC]], [BT[XCB]])
            xcb3 = xcb.rearrange("p (k n) -> p k n", k=1)
            br = proj_fm(None, 1, xcb3, [BT[XCB]], wview=lwa[:, j:j + 1, :])
            bi = proj_fm(None, 1, xcb3, [BT[XCB]], wview=lwx[:, j:j + 1, :])
            act(lambda e: e.activation(v2(T[R]), psv(br), AF.Sigmoid, bias=cpar[:, j, BA:BA + 1]),
                [B_ps[br], B_ps[br + 1], B_const], [BT[R]])
            act(lambda e: e.activation(v2(T[I]), psv(bi), AF.Sigmoid, bias=cpar[:, j, BX:BX + 1]),
                [B_ps[bi], B_ps[bi + 1], B_const], [BT[I]])
            act(lambda e: e.activation(T[Aa], T[R], AF.Exp, scale=cA[:, j:j + 1]), [BT[R], B_const], [BT[Aa]])
            act(lambda e: e.activation(T[Bt], T[R], AF.Exp, scale=cA2[:, j:j + 1]), [BT[R], B_const], [BT[Bt]])
        if part == 1:
            return
        dve(lambda e: e.tensor_scalar(T[Bt], T[Bt], -1.0, 1.0, ALU.mult, ALU.add), [BT[Bt]], [BT[Bt]])
        dve(lambda e: e.tensor_scalar(T[Bt], T[Bt], 0.0, None, ALU.max), [BT[Bt]], [BT[Bt]])
        act(lambda e: e.activation(T[Bt], T[Bt], AF.Sqrt), [BT[Bt]], [BT[Bt]])
        dve(lambda e: e.tensor_tensor(T[Bt], T[Bt], T[I], ALU.mult), [BT[Bt], BT[I]], [BT[Bt]])
        dve(lambda e: e.tensor_tensor(T[Bt], T[Bt], xc, ALU.mult), [BT[Bt], BT[XC]], [BT[Bt]])
        dve(lambda e: e.memset(T[Aa][:, 2:3], 0.0), [], [BT[Aa]])
        if P["init"] == "zero":
            dve(lambda e: e.memset(T[Bt][:, 2:3], 0.0), [], [BT[Bt]])
        else:
            dve(lambda e: e.tensor_copy(T[Bt][:, 2:3], hcar[:, j:j + 1]), [B_hcar], [BT[Bt]])
        if P["samples"]:
            av = T[Aa][:, SB0:SB0 + 176].rearrange("p (b s) -> p b s", s=11)[:, :, 2:3]
            bv = T[Bt][:, SB0:SB0 + 176].rearrange("p (b s) -> p b s", s=11)[:, :, 2:3]
            dve(lambda e: e.memset(av, 0.0), [], [BT[Aa]])
            dve(lambda e: e.tensor_copy(bv, shT[:, j, :].rearrange("p (b o) -> p b o", o=1)),
                [B_const], [BT[Bt]])
        dve(lambda e: e.tensor_tensor_scan(T[H], T[Aa], T[Bt], 0.0, ALU.mult, ALU.add),
            [BT[Aa], BT[Bt]], [BT[H]])
        lc = P["last"]
        if P["carry"] == "flag":
            dve(lambda e: e.tensor_scalar(hcar[:, j:j + 1], T[H][:, lc:lc + 1], flag[:, 0:1], None, ALU.mult),
                [BT[H], B_const], [B_hcar])
        elif P["carry"] == "plain":
            dve(lambda e: e.tensor_copy(hcar[:, j:j + 1], T[H][:, lc:lc + 1]), [BT[H]], [B_hcar])
        if P["samples"]:
            xv = T[XR][:, SB0:SB0 + 176].rearrange("p (b s) -> p b s", s=11)[:, :, 8:11]
            hv = T[H][:, SB0:SB0 + 176].rearrange("p (b s) -> p b s", s=11)[:, :, 10:11]
            dve(lambda e: e.tensor_copy(oxr[:, j, 0:3], T[XR][:, lc - 2:lc + 1]), [BT[XR]], [B_oxr])
            dve(lambda e: e.tensor_copy(oxr[:, j, 3:51].rearrange("p (b s) -> p b s", s=3), xv),
                [BT[XR]], [B_oxr])
            dve(lambda e: e.tensor_copy(ohh[:, j, 0:1], T[H][:, lc:lc + 1]), [BT[H]], [B_ohh])
            dve(lambda e: e.tensor_copy(ohh[:, j, 1:17].rearrange("p (b o) -> p b o", o=1), hv),
                [BT[H]], [B_ohh])
        if not P["main"]:
            return
        dve(lambda e: e.tensor_tensor(T[G], T[Y], T[Y], ALU.mult), [BT[Y]], [BT[G]])
        dve(lambda e: e.tensor_scalar(T[G], T[G], 0.044715, 1.0, ALU.mult, ALU.add), [BT[G]], [BT[G]])
        dve(lambda e: e.tensor_tensor(T[G], T[G], T[Y], ALU.mult), [BT[G], BT[Y]], [BT[G]])
        act(lambda e: e.activation(T[G], T[G], AF.Sigmoid, scale=1.5957691216057308), [BT[G]], [BT[G]])
        dve(lambda e: e.tensor_tensor(T[G], T[G], T[Y], ALU.mult), [BT[G], BT[Y]], [BT[G]])
        dve(lambda e: e.tensor_tensor(orT[:, j, :], T[H], T[G], ALU.mult), [BT[H], BT[G]], [B_or[j]])

    def mixer_a(j, bb_, bv_, P):
        T = tmpA
        BT = B_tA
        C, CV, CA = 10, 11, 12
        dve(lambda e: e.tensor_tensor(v2(T[CV]), v2(T[C]), psv(bv_), ALU.mult),
            [BT[C], B_ps[bv_], B_ps[bv_ + 1]], [BT[CV]])
        cv, ca = T[CV], T[CA]
        if P["samples"]:
            dve(lambda e: e.tensor_copy(
                cv[:, SB0:SB0 + 176].rearrange("p (b s) -> p b s", s=11)[:, :, 1:3], saT[:, j, :, :]),
                [B_const], [BT[CV]])
        dve(lambda e: e.memset(ca[:, 0:2], 0.0), [], [BT[CA]])
        dve(lambda e: e.tensor_scalar(ca[:, 2:N], cv[:, 2:N], cpar[:, j, CW_A + 2:CW_A + 3], None, ALU.mult),
            [BT[CV], B_const], [BT[CA]])
        for t in range(2):
            dve(lambda e, t=t: e.scalar_tensor_tensor(
                ca[:, 2:N], cv[:, t:N - 2 + t], cpar[:, j, CW_A + t:CW_A + t + 1], ca[:, 2:N],
                ALU.mult, ALU.add), [BT[CV], BT[CA], B_const], [BT[CA]])
        dve(lambda e: e.tensor_tensor(v2(oaT[:, j, :]), v2(ca), psv(bb_), ALU.mult),
            [BT[CA], B_ps[bb_], B_ps[bb_ + 1]], [B_oa[j]])
        if P["samples"]:
            lc = P["last"]
            cvv = cv[:, SB0:SB0 + 176].rearrange("p (b s) -> p b s", s=11)[:, :, 9:11]
            dve(lambda e: e.tensor_copy(ocv[:, j, 0:2], cv[:, lc - 1:lc + 1]), [BT[CV]], [B_ocv])
            dve(lambda e: e.tensor_copy(ocv[:, j, 2:34].rearrange("p (b s) -> p b s", s=2), cvv),
                [BT[CV]], [B_ocv])

    OFFS = [0, 128, 256, 384, 512]

    def phase_L(src_d, row0):
        def prep(g):
            gs = GS[g]
            bi = g % 2
            s0 = OFFS[g]
            S.dma("sp", xg[bi][:gs, :], src_d[row0 + s0:row0 + s0 + gs, :], B_xg[bi], writes=[B_xg[bi]])
            rms_rstd(xg[bi][:gs, :], gs, xsL[bi], B_xg[bi], B_xsL[bi], g)
            act(lambda e: e.activation(xsL[bi][:gs, :], xg[bi][:gs, :], AF.Copy, scale=small[:gs, g:g + 1]),
                [B_xg[bi], B_sm[g]], [B_xsL[bi]])
        prep(0)
        for g in range(NG):
            if g + 1 < NG:
                prep(g + 1)
            transpose_group(xsL[g % 2], B_xsL[g % 2], GS[g], OFFS[g], uT, B_uT, 0, False)

    import os
    STOP = int(os.environ.get("MK_STOP", "99"))
    cnt = [0]

    def stop():
        cnt[0] += 1
        return cnt[0] > STOP

    def run_pass(P):
        if stop():
            return
        S.barrier()
        phase_L(P["src"], P["row0"])
        S.barrier()
        if stop():
            return
        if not P["main"]:
            XRS = [0, 8]
            SETS = [tuple(range(0, 8)), tuple(range(8, 16))]

            def proj_x(j):
                sx = w_in_chunk(48 + j)
                bx = proj_fm(sx, KD, uT, B_uT)
                copy_x(bx, XRS[j % 2])
            proj_x(0)
            proj_x(1)
            mixer_r(0, P, idx=SETS[0], part=1)
            for j in range(NCH):
                if j + 2 < NCH:
                    proj_x(j + 2)
                if j + 1 < NCH:
                    mixer_r(j + 1, P, idx=SETS[(j + 1) % 2], part=1)
                mixer_r(j, P, idx=SETS[j % 2], part=2)
            return
        for j in range(NCH):
            sx = w_in_chunk(48 + j)
            bx = proj_fm(sx, KD, uT, B_uT)
            sy = w_in_chunk(64 + j)
            by = proj_fm(sy, KD, uT, B_uT)
            sc = w_in_chunk(16 + j)
            bc_ = proj_fm(sc, KD, uT, B_uT)
            copy_x(bx, 0)
            copy_y(by)
            copy_c(bc_)
            mixer_r(j, P)
            sv = w_in_chunk(32 + j)
            bv_ = proj_fm(sv, KD, uT, B_uT)
            sbb = w_in_chunk(j)
            bb_ = proj_fm(sbb, KD, uT, B_uT)
            mixer_a(j, bb_, bv_, P)
        if not P["main"]:
            return
        if stop():
            return
        S.barrier()
        for m in range(KD):
            sga = w_in_chunk(80 + m)
            bga = proj_fm(sga, KD, uT, B_uT)
            act(lambda e, b=bga: e.activation(v2(tmpB[0]), psv(b), AF.Sigmoid),
                [B_ps[bga], B_ps[bga + 1]], [B_tB[0]])
            sgr = w_in_chunk(112 + m)
            bgr = proj_fm(sgr, KD, uT, B_uT)
            act(lambda e, b=bgr: e.activation(v2(tmpB[1]), psv(b), AF.Sigmoid),
                [B_ps[bgr], B_ps[bgr + 1]], [B_tB[1]])
            sbr = wload([(lambda r: r[:, 0:2048].rearrange("p (k c) -> p k c", c=128),
                          w_bra[:, m * 128:(m + 1) * 128].rearrange("(k p) c -> p k c", p=128)),
                         (lambda r: r[:, 2048:4096].rearrange("p (k c) -> p k c", c=128),
                          w_brr[:, m * 128:(m + 1) * 128].rearrange("(k p) c -> p k c", p=128))])
            wva = ring[:, sbr, 0:2048].rearrange("p (k c) -> p k c", c=128)
            wvr = ring[:, sbr, 2048:4096].rearrange("p (k c) -> p k c", c=128)
            bpa = ps_alloc(2)

            def mma(e, b=bpa, wv=wva):
                last = None
                for k in range(NCH):
                    for nt in range(2):
                        last = e.matmul(ps[:, b + nt, 0:NT], wv[:, k, :], oaT[:, k, nt * NT:(nt + 1) * NT],
                                        start=(k == 0), stop=(k == NCH - 1))
                return last
            pe(mma, B_oa + [B_ring[sbr]], [B_ps[bpa], B_ps[bpa + 1]])
            dve(lambda e, b=bpa: e.tensor_tensor(v2(tmpB[2]), v2(tmpB[0]), psv(b), ALU.mult),
                [B_tB[0], B_ps[bpa], B_ps[bpa + 1]], [B_tB[2]])
            bpr = ps_alloc(2)

            def mmr(e, b=bpr, wv=wvr):
                last = None
                for k in range(NCH):
                    for nt in range(2):
                        last = e.matmul(ps[:, b + nt, 0:NT], wv[:, k, :], orT[:, k, nt * NT:(nt + 1) * NT],
                                        start=(k == 0), stop=(k == NCH - 1))
                return last
            pe(mmr, B_or + [B_ring[sbr]], [B_ps[bpr], B_ps[bpr + 1]])
            dve(lambda e, b=bpr: e.tensor_tensor(v2(tmpB[3]), v2(tmpB[1]), psv(b), ALU.mult),
                [B_tB[1], B_ps[bpr], B_ps[bpr + 1]], [B_tB[3]])
            dve(lambda e, m=m: e.tensor_tensor(mT[:, m, :], tmpB[2], tmpB[3], ALU.add),
                [B_tB[2], B_tB[3]], [B_mT[m]])
        if stop():
            return
        S.barrier()
        s0 = 0
        for g in range(NG):
            gs = GS[g]
            S.dma("sp", x2[:gs, g, :], P["src"][P["row0"] + s0:P["row0"] + s0 + gs, :], B_x2[g],
                  writes=[B_x2[g]])
            s0 += gs
        for blk in range(8):
            slots = []
            for kg in range(4):
                slots.append(wload([(v3(8), w_o[kg * 1024:(kg + 1) * 1024, blk * 512:(blk + 1) * 512]
                                     .rearrange("(k p) c -> p k c", p=128))]))
            banks = [ps_alloc(1) for _ in range(NG)]
            for kg in range(4):
                wv = ring[:, slots[kg], :].rearrange("p (k c) -> p k c", k=8)
                s0 = 0
                for g in range(NG):
                    gs = GS[g]

                    def mmc(e, kg=kg, wv=wv, bank=banks[g], gs=gs, s0=s0):
                        last = None
                        for kk in range(8):
                            k = kg * 8 + kk
                            last = e.matmul(ps[:gs, bank, :], mT[:, k, s0:s0 + gs], wv[:, kk, :],
                                            start=(k == 0), stop=(k == KD - 1))
                        return last
                    pe(mmc, B_mT[kg * 8:(kg + 1) * 8] + [B_ring[slots[kg]]], [B_ps[banks[g]]])
                    s0 += gs
            for g in range(NG):
                gs = GS[g]
                dve(lambda e, g=g, gs=gs, b=banks[g], blk=blk: e.tensor_tensor(
                    x2[:gs, g, blk * 512:(blk + 1) * 512], x2[:gs, g, blk * 512:(blk + 1) * 512],
                    ps[:gs, b, :], ALU.add), [B_x2[g], B_ps[banks[g]]], [B_x2[g]])
        if stop():
            return
        S.barrier()
        def prep_n(g):
            gs = GS[g]
            bi = g % 2
            rms_rstd(x2[:gs, g, :], gs, xsX[bi], B_x2[g], B_xsX[bi], 8 + g)
            act(lambda e: e.activation(xsX[bi][:gs, :], x2[:gs, g, :], AF.Copy, scale=small[:gs, 8 + g:9 + g]),
                [B_x2[g], B_sm[8 + g]], [B_xsX[bi]])
        prep_n(0)
        for g in range(NG):
            gs = GS[g]
            s0 = OFFS[g]
            if g + 1 < NG:
                prep_n(g + 1)
            transpose_group(xsX[g % 2], B_xsX[g % 2], gs, s0, mT, B_mT, 1, False)
            bl = ps_alloc(1)

            def mmr2(e, gs=gs, s0=s0, bl=bl):
                last = None
                for k in range(KD):
                    last = e.matmul(ps[:gs, bl, 0:36], mT[:, k, s0:s0 + gs], wr[:, k, :],
                                    start=(k == 0), stop=(k == KD - 1))
                return last
            pe(mmr2, B_mT + [B_cp], [B_ps[bl]])
            routing(g, gs, bl)
        if stop():
            return
        S.barrier()
        def moe_gu(ex, hc):
            hb = ex % 2
            sg = wload([(lambda r: r, w_gate[ex, hc])])
            bg = proj_fm(sg, KD, mT, B_mT)
            act(lambda e, b=bg, hc=hc: e.activation(v2(slT[hc % 2]), psv(b), AF.Silu),
                [B_ps[bg], B_ps[bg + 1]], [B_sl[hc % 2]])
            su = wload([(lambda r: r, w_up[ex, hc])])
            bu = proj_fm(su, KD, mT, B_mT)
            dve(lambda e, b=bu, hc=hc, hb=hb: e.tensor_tensor(
                v2(hsT[hb][:, hc, :]), v2(slT[hc % 2]), psv(b), ALU.mult),
                [B_sl[hc % 2], B_ps[bu], B_ps[bu + 1]], [B_hs[hb]])

        def moe_down(ex):
            hb = ex % 2
            for b2 in range(4):
                sd = wload([(v3(4), w_down[ex, :, b2 * 1024:(b2 + 1) * 1024].rearrange("(k p) c -> p k c", p=128))])
                wv = ring[:, sd, :].rearrange("p (k c) -> p k c", k=4)
                s0 = 0
                for g in range(NG):
                    gs = GS[g]
                    b = ps_alloc(2)

                    def mmd(e, wv=wv, b=b, hb=hb, gs=gs, s0=s0):
                        last = None
                        for k in range(4):
                            for half in range(2):
                                last = e.matmul(ps[:gs, b + half, :], hsT[hb][:, k, s0:s0 + gs],
                                                wv[:, k, half * 512:(half + 1) * 512],
                                                start=(k == 0), stop=(k == 3))
                        return last
                    pe(mmd, [B_hs[hb], B_ring[sd]], [B_ps[b], B_ps[b + 1]])
                    dve(lambda e, g=g, gs=gs, b=b, b2=b2, ex=ex: e.scalar_tensor_tensor(
                        x2[:gs, g, b2 * 1024:(b2 + 1) * 1024].rearrange("p (a c) -> p a c", a=2),
                        ps[:gs, b:b + 2, :], gates[:gs, g, ex:ex + 1],
                        x2[:gs, g, b2 * 1024:(b2 + 1) * 1024].rearrange("p (a c) -> p a c", a=2),
                        ALU.mult, ALU.add),
                        [B_x2[g], B_ps[b], B_ps[b + 1], B_gates[g]], [B_x2[g]])
                    s0 += gs

        for hc in range(4):
            moe_gu(0, hc)
        for ex in range(NE):
            if ex + 1 < NE:
                moe_gu(ex + 1, 0)
            moe_down(ex)
            if ex + 1 < NE:
                for hc in range(1, 4):
                    moe_gu(ex + 1, hc)
        if stop():
            return
        S.barrier()
        S.dma("sp", nfbc, nf_d, B_nf, writes=[B_nf])
        s0 = 0
        for g in range(NG):
            gs = GS[g]
            rms_rstd(x2[:gs, g, :], gs, jxX, B_x2[g], B_jxX, 16 + g)
            dve(lambda e, gs=gs, g=g: e.scalar_tensor_tensor(
                x2[:gs, g, :], x2[:gs, g, :], small[:gs, 16 + g:17 + g], nfbc[:gs, :], ALU.mult, ALU.mult),
                [B_x2[g], B_sm[16 + g], B_nf], [B_x2[g]])
            S.dma("sp", y_d[P["row0"] + s0:P["row0"] + s0 + gs, :], x2[:gs, g, :], B_x2[g], reads=[B_x2[g]])
            s0 += gs

    def routing(g, gs, bl):
        R_ = rt
        lg = R_[:gs, 0:36]
        gl = R_[:gs, 0:4]
        rd = [B_rt]
        wrt = [B_rt]
        dve(lambda e: e.tensor_tensor(lg, ps[:gs, bl, 0:36], rbias[:gs, :], ALU.add), [B_ps[bl], B_const], wrt)
        gmax = R_[:gs, 40:41]
        dve(lambda e: e.tensor_reduce(gmax, gl, AX.X, ALU.max), rd, wrt)
        ngm = R_[:gs, 41:42]
        dve(lambda e: e.tensor_scalar(ngm, gmax, -1.0, None, ALU.mult), rd, wrt)
        ge = R_[:gs, 44:48]
        act(lambda e: e.activation(ge, gl, AF.Exp, bias=ngm, scale=1.0), rd, wrt)
        gsum = R_[:gs, 42:43]
        dve(lambda e: e.tensor_reduce(gsum, ge, AX.X, ALU.add), rd, wrt)
        gp = R_[:gs, 43:44]
        dve(lambda e: e.reciprocal(gp, gsum), rd, wrt)
        ohg = R_[:gs, 48:52]
        dve(lambda e: e.tensor_scalar(ohg, gl, gmax, None, ALU.is_equal), rd, wrt)
        esel = R_[:gs, 56:64]
        dve(lambda e: e.tensor_scalar(esel, R_[:gs, 4:12], ohg[:, 0:1], None, ALU.mult), rd, wrt)
        for q in range(1, 4):
            dve(lambda e, q=q: e.scalar_tensor_tensor(esel, R_[:gs, 4 + 8 * q:12 + 8 * q], ohg[:, q:q + 1], esel,
                                                      ALU.mult, ALU.add), rd, wrt)
        m1 = R_[:gs, 64:65]
        dve(lambda e: e.tensor_reduce(m1, esel, AX.X, ALU.max), rd, wrt)
        oh1 = R_[:gs, 72:80]
        dve(lambda e: e.tensor_scalar(oh1, esel, m1, None, ALU.is_equal), rd, wrt)
        e2 = R_[:gs, 80:88]
        dve(lambda e: e.scalar_tensor_tensor(e2, oh1, -1.0e30, esel, ALU.mult, ALU.add), rd, wrt)
        m2 = R_[:gs, 65:66]
        dve(lambda e: e.tensor_reduce(m2, e2, AX.X, ALU.max), rd, wrt)
        oh2 = R_[:gs, 88:96]
        dve(lambda e: e.tensor_scalar(oh2, e2, m2, None, ALU.is_equal), rd, wrt)
        dd = R_[:gs, 66:67]
        dve(lambda e: e.tensor_tensor(dd, m2, m1, ALU.subtract), rd, wrt)
        ed = R_[:gs, 67:68]
        act(lambda e: e.activation(ed, dd, AF.Exp), rd, wrt)
        den = R_[:gs, 68:69]
        dve(lambda e: e.tensor_scalar(den, ed, 1.0, None, ALU.add), rd, wrt)
        rden = R_[:gs, 69:70]
        dve(lambda e: e.reciprocal(rden, den), rd, wrt)
        w1 = R_[:gs, 70:71]
        dve(lambda e: e.tensor_tensor(w1, rden, gp, ALU.mult), rd, wrt)
        w2 = R_[:gs, 71:72]
        dve(lambda e: e.tensor_tensor(w2, w1, ed, ALU.mult), rd, wrt)
        g8 = R_[:gs, 96:104]
        dve(lambda e: e.tensor_scalar(g8, oh1, w1, None, ALU.mult), rd, wrt)
        dve(lambda e: e.scalar_tensor_tensor(g8, oh2, w2, g8, ALU.mult, ALU.add), rd, wrt)
        for q in range(4):
            dve(lambda e, q=q: e.tensor_scalar(gates[:gs, g, 8 * q:8 * q + 8], g8, ohg[:, q:q + 1], None, ALU.mult),
                rd, [B_gates[g]])

    run_pass(dict(src=xpre, row0=0, main=False, samples=False, init="zero", carry="plain", last=N - 1))
    run_pass(dict(src=xpre, row0=N, main=False, samples=False, init="carry", carry="flag", last=SB0 - 1))
    run_pass(dict(src=xin, row0=0, main=True, samples=False, init="carry", carry="plain", last=N - 1))
    run_pass(dict(src=xin, row0=N, main=True, samples=True, init="carry", carry="none", last=SB0 - 1))
    if cnt[0] <= STOP:
        S.dma("sp", ocv_d, ocv[:], B_ocv, reads=[B_ocv])
        S.dma("sp", oxr_d, oxr[:], B_oxr, reads=[B_oxr])
        S.dma("sp", oh_d, ohh[:], B_ohh, reads=[B_ohh])
    sp = S.engs["sp"]
    for ev in S.pending_dma:
        S._wait(sp, ev)
    S.barrier()

    with nc.Block() as block:
        @block.tensor
        def _(t):
            S.replay("pe", t)

        @block.scalar
        def _(a):
            S.replay("act", a)

        @block.vector
        def _(v):
            S.replay("dve", v)

        @block.sync
        def _(s):
            S.replay("sp", s)

        @block.gpsimd
        def _(g):
            S.replay("pool", g)


_NC_CACHE = {}


def _get_nc():
    if "nc" not in _NC_CACHE:
        _NC_CACHE["nc"] = build_program()
    return _NC_CACHE["nc"]


def _chunked(v, nchunk):
    return np.ascontiguousarray(np.moveaxis(v.reshape(v.shape[:-1] + (nchunk, 128)), -1, 0))


def _gu_layout(w):
    e = w.shape[0]
    return np.ascontiguousarray(w.reshape(e, KD, 128, 4, 128).transpose(0, 3, 2, 1, 4)).reshape(e, 4, 128, GRAN)


def kernel(x_prompt, x_sample, state_conv_a, state_conv_r, state_h, meta_tokens, norm1, w_in, conv_a_w, conv_r_w,
           conv_r_b, lru_wa, lru_ba, lru_wx, lru_bx, lru_lam, w_br_a, w_br_r, w_o, norm2, w_group, b_group,
           w_router, b_router, w_gate, w_up, w_down, norm_f):
    f = np.float32
    x_prompt = np.asarray(x_prompt, f)
    x_sample = np.asarray(x_sample, f)
    H = 1032
    cp = np.zeros((12, W), f)
    cp[0:3] = np.asarray(conv_a_w, f)[0]
    cp[3:7] = np.asarray(conv_r_w, f)[0]
    cp[7] = np.asarray(conv_r_b, f)[0]
    cp[8] = np.asarray(lru_ba, f)[0]
    cp[9] = np.asarray(lru_bx, f)[0]
    cp[10] = np.asarray(lru_lam, f)[0]
    cpar = np.ascontiguousarray(cp.reshape(12, NCH, 128).transpose(2, 1, 0))
    ncols = np.ascontiguousarray(
        np.stack([np.asarray(norm1, f)[0], np.asarray(norm2, f)[0]]).reshape(2, KD, 128).transpose(2, 0, 1))
    wr = np.ascontiguousarray(np.concatenate([np.asarray(w_group, f)[0], np.asarray(w_router, f)[0]], axis=1))
    rbias = np.ascontiguousarray(np.broadcast_to(np.concatenate([np.asarray(b_group, f)[0], np.asarray(b_router, f)[0]])[None, :], (128, 36)))
    shared = {
        "w_in": np.ascontiguousarray(np.asarray(w_in, f)[0].reshape(KD, 128, 144, 128).transpose(2, 1, 0, 3))
        .reshape(144, 128, GRAN), "w_br_a": np.asarray(w_br_a, f)[0], "w_br_r": np.asarray(w_br_r, f)[0],
        "w_o": np.asarray(w_o, f)[0], "w_gate": _gu_layout(np.asarray(w_gate, f)[0]), "w_up": _gu_layout(np.asarray(w_up, f)[0]),
        "w_down": np.asarray(w_down, f)[0], "lru_wa": np.asarray(lru_wa, f)[0], "lru_wx": np.asarray(lru_wx, f)[0],
        "wr": wr, "rbias": rbias, "cpar": cpar, "ncols": ncols, "normf": np.ascontiguousarray(np.broadcast_to(np.asarray(norm_f, f)[None, :], (128, D))),
        "ident": np.eye(128, dtype=f),
    }
    meta = np.asarray(meta_tokens, f)
    sca = np.asarray(state_conv_a, f)[0]
    scr = np.asarray(state_conv_r, f)[0]
    sth = np.asarray(state_h, f)[0]
    in_maps = []
    for c in range(NCORES):
        s, half = c // 2, c % 2
        full = np.concatenate([meta, x_prompt[s]], axis=0)
        xpre = np.zeros((2 * N, D), f)
        xin = np.zeros((2 * N, D), f)
        if half == 1:
            xpre[3:N] = full[0:605]
            xpre[N:N + 3] = full[602:605]
            xpre[N + 3:N + SB0] = full[605:H]
            xin[0:3] = full[H - 3:H]
        base = half * H
        xin[3:N] = full[base:base + 605]
        xin[N:N + 3] = full[base + 602:base + 605]
        xin[N + 3:N + SB0] = full[base + 605:base + H]
        for bl in range(16):
            r0 = N + SB0 + 11 * bl + 3
            xin[r0:r0 + 8] = x_sample[16 * c + bl]
        sa = sca[16 * c:16 * c + 16]
        sr = scr[16 * c:16 * c + 16]
        sh = sth[16 * c:16 * c + 16]
        m = dict(shared)
        m.update({
            "xpre": xpre, "xin": xin,
            "sa": np.ascontiguousarray(sa.reshape(16, 2, NCH, 128).transpose(3, 2, 0, 1)),
            "sr": np.ascontiguousarray(sr.reshape(16, 3, NCH, 128).transpose(3, 2, 0, 1)),
            "sh": np.ascontiguousarray(sh.reshape(16, NCH, 128).transpose(2, 1, 0)),
            "flag": np.full((128, 1), float(half), f),
        })
        in_maps.append(m)
    import os
    if os.environ.get("MK_TINY"):
        for m in in_maps:
            for k in ("w_br_a", "w_br_r", "w_o"):
                m[k] = m[k][:128]
    if os.environ.get("MK_SMALL"):
        for m in in_maps:
            for k in ("w_gate", "w_up", "w_down"):
                m[k] = m[k][0:1]
    nc = _get_nc()
    res = run_bass_kernel_spmd(nc, in_maps, core_ids=list(range(NCORES)))
    R = res.results
    B, SEQ = x_prompt.shape[0], x_prompt.shape[1]
    y_prompt = np.zeros((B, SEQ, D), f)
    y_sample = np.zeros((x_sample.shape[0], 8, D), f)
    pa = np.zeros((1, B, 2, W), f)
    pr = np.zeros((1, B, 3, W), f)
    ph = np.zeros((1, B, W), f)
    sa_o = np.zeros((1, 128, 2, W), f)
    sr_o = np.zeros((1, 128, 3, W), f)
    sh_o = np.zeros((1, 128, W), f)

    def unchunk(a):
        return a.transpose(2, 1, 0).reshape(a.shape[2], W)
    for c in range(NCORES):
        s, half = c // 2, c % 2
        y = R[c]["y"]
        rows = np.concatenate([y[3:N], y[N + 3:N + SB0]], axis=0)
        if half == 0:
            y_prompt[s, 0:H - 16] = rows[16:]
        else:
            y_prompt[s, H - 16:] = rows
        ys = y[N + SB0:N + SB0 + 176].reshape(16, 11, D)[:, 3:, :]
        y_sample[16 * c:16 * c + 16] = ys
        cv = unchunk(R[c]["o_cv"])
        xr = unchunk(R[c]["o_xr"])
        hh = unchunk(R[c]["o_h"])
        if half == 1:
            pa[0, s] = cv[0:2]
            pr[0, s] = xr[0:3]
            ph[0, s] = hh[0]
        sa_o[0, 16 * c:16 * c + 16] = cv[2:].reshape(16, 2, W)
        sr_o[0, 16 * c:16 * c + 16] = xr[3:].reshape(16, 3, W)
        sh_o[0, 16 * c:16 * c + 16] = hh[1:]
    return (y_prompt, y_sample, pa, pr, ph, sa_o, sr_o, sh_o)
```
